# Optimizing a Trainium2 kernel written in Bass

```python
import math
import jax, jax.numpy as jnp
from jax import lax
import numpy as np

D_MODEL = 1024
BATCH = 16
SEQ = 2048
DEPTH = 2

N_MIXERS = 2
N_A_LAYERS = (DEPTH + N_MIXERS - 1) // N_MIXERS
N_B_LAYERS = DEPTH // N_MIXERS

A_HEADS = D_MODEL // 128
A_HEAD_DIM = 128
A_V_DIM = 128
Q_RANK = 256
KV_RANK = 256
IDX_HEADS = 8
IDX_DIM = 64
TOPK_MAX = 256
Q_BLOCK = 128
A_IN = Q_RANK + KV_RANK + IDX_DIM + IDX_HEADS

NUM_BUCKETS = 32
MAX_DISTANCE = 128

G_HEADS = 4
G_KD = D_MODEL // 2
G_VD = D_MODEL
G_DK = G_KD // G_HEADS
G_DV = G_VD // G_HEADS
G_RANK = 16
GATE_TAU = 16.0
CHUNK = 64
B_IN = 2 * G_KD + 2 * G_VD + G_RANK

D_FF = 2816
CONV_WIDTH = 3

EPS = 1e-6

kernel_name = "hybrid_dsa_gla_convffn"


def rmsnorm(x, g):
    xf = x.astype(jnp.float32)
    y = xf * lax.rsqrt(jnp.mean(xf * xf, axis=-1, keepdims=True) + EPS)
    return (y * g.astype(jnp.float32)).astype(x.dtype)


def t5_bucket(dist):
    n = jnp.maximum(dist, 0)
    exact = NUM_BUCKETS // 2
    log_ratio = jnp.log(jnp.maximum(n, 1).astype(jnp.float32) / exact) / math.log(MAX_DISTANCE / exact)
    large = exact + (log_ratio * (NUM_BUCKETS - exact)).astype(jnp.int32)
    return jnp.where(n < exact, n, jnp.minimum(large, NUM_BUCKETS - 1))


def dsa_mixer(h, rel_bias, w_in, g_cq, g_ckv, w_uq, w_uk, w_uv, w_qi, w_o):
    B, T, _ = h.shape
    c_q, c_kv, k_idx, w_idx = jnp.split(
        h @ w_in, [Q_RANK, Q_RANK + KV_RANK, Q_RANK + KV_RANK + IDX_DIM], axis=-1)
    c_q = rmsnorm(c_q, g_cq)
    c_kv = rmsnorm(c_kv, g_ckv)
    q = (c_q @ w_uq).reshape(B, T, A_HEADS, A_HEAD_DIM)
    q_lat = jnp.einsum('bthd,hdc->bthc', q, w_uk) * (A_HEAD_DIM ** -0.5)
    q_idx = (c_q @ w_qi).reshape(B, T, IDX_HEADS, IDX_DIM)
    w_idx = w_idx * (IDX_HEADS ** -0.5 * IDX_DIM ** -0.5)

    n_sel = min(TOPK_MAX, T // 4)
    n_blk = T // Q_BLOCK
    key_pos = jnp.arange(T, dtype=jnp.int32)

    def to_blocks(a):
        return a.reshape(B, n_blk, Q_BLOCK, *a.shape[2:]).swapaxes(0, 1)

    def attend_block(args):
        qb_lat, qb_idx, wb, start = args
        q_pos = start + jnp.arange(Q_BLOCK, dtype=jnp.int32)
        rel = jax.nn.relu(jnp.einsum('bqhd,bsd->bqhs', qb_idx, k_idx))
        score = jnp.einsum('bqh,bqhs->bqs', wb, rel).astype(jnp.float32)
        score = jnp.where(key_pos[None, None, :] <= q_pos[None, :, None], score, -jnp.inf)
        _, sel = lax.top_k(score, n_sel)
        kv_sel = jax.vmap(lambda a, i: a[i])(c_kv, sel.reshape(B, -1))
        kv_sel = kv_sel.reshape(B, Q_BLOCK, n_sel, KV_RANK)
        dist = q_pos[None, :, None] - sel
        bias = rel_bias[t5_bucket(dist)].transpose(0, 1, 3, 2)
        logits = (jnp.einsum('bqhc,bqkc->bqhk', qb_lat, kv_sel).astype(jnp.float32)
                  + bias.astype(jnp.float32))
        logits = jnp.where((dist >= 0)[:, :, None, :], logits, -jnp.inf)
        p = jax.nn.softmax(logits, axis=-1).astype(kv_sel.dtype)
        return jnp.einsum('bqhk,bqkc->bqhc', p, kv_sel)

    starts = jnp.arange(n_blk, dtype=jnp.int32) * Q_BLOCK
    o_lat = lax.map(attend_block, (to_blocks(q_lat), to_blocks(q_idx), to_blocks(w_idx), starts))
    o_lat = o_lat.swapaxes(0, 1).reshape(B, T, A_HEADS, KV_RANK)
    o = jnp.einsum('bthc,hcv->bthv', o_lat, w_uv).reshape(B, T, A_HEADS * A_V_DIM)
    return o @ w_o


def gla_mixer(h, w_in, w_g2, b_g, g_norm, w_o):
    B, T, _ = h.shape
    q, k, v, r, g_lr = jnp.split(
        h @ w_in, [G_KD, 2 * G_KD, 2 * G_KD + G_VD, 2 * G_KD + 2 * G_VD], axis=-1)
    log_a = jax.nn.log_sigmoid((g_lr @ w_g2 + b_g).astype(jnp.float32)) / GATE_TAU
    n_chk = T // CHUNK

    def heads_chunks(a, d):
        return a.astype(jnp.float32).reshape(B, n_chk, CHUNK, G_HEADS, d).transpose(1, 0, 3, 2, 4)

    qc = heads_chunks(q * (G_DK ** -0.5), G_DK)
    kc = heads_chunks(k, G_DK)
    vc = heads_chunks(v, G_DV)
    gc = heads_chunks(log_a, G_DK)
    idx = jnp.arange(CHUNK)
    causal = (idx[:, None] >= idx[None, :])[None, None, :, :, None]

    def step(S, inp):
        q_c, k_c, v_c, g_c = inp
        b = jnp.cumsum(g_c, axis=2)
        o_inter = jnp.einsum('bhik,bhkv->bhiv', q_c * jnp.exp(b), S)
        diff = jnp.where(causal, b[:, :, :, None, :] - b[:, :, None, :, :], -jnp.inf)
        attn = jnp.einsum('bhik,bhjk,bhijk->bhij', q_c, k_c, jnp.exp(diff))
        o_intra = jnp.einsum('bhij,bhjv->bhiv', attn, v_c)
        b_last = b[:, :, -1:, :]
        S = (S * jnp.exp(b_last[:, :, 0, :])[..., None]
             + jnp.einsum('bhjk,bhjv->bhkv', k_c * jnp.exp(b_last - b), v_c))
        return S, o_inter + o_intra

    S0 = jnp.zeros((B, G_HEADS, G_DK, G_DV), jnp.float32)
    _, o = lax.scan(step, S0, (qc, kc, vc, gc))
    o = o.transpose(1, 0, 3, 2, 4).reshape(B, T, G_HEADS, G_DV)
    o = rmsnorm(o, g_norm.reshape(G_HEADS, G_DV)).reshape(B, T, G_VD).astype(h.dtype)
    return (o * jax.nn.silu(r)) @ w_o


def conv_ffn(h, w_in, conv_w, conv_b, w_out):
    u, v = jnp.split(h @ w_in, 2, axis=-1)
    u = lax.conv_general_dilated(
        u, conv_w[:, None, :], window_strides=(1,), padding=[(CONV_WIDTH - 1, 0)],
        dimension_numbers=('NWC', 'WIO', 'NWC'), feature_group_count=D_FF) + conv_b
    return (jax.nn.gelu(u, approximate=False) * v) @ w_out


def setup_inputs(seed: int = 0) -> dict:
    key = jax.random.key(seed)
    ks = iter(jax.random.split(key, 40))
    f32 = jnp.float32

    def w(shape, fan_in):
        return jax.random.normal(next(ks), shape, f32) * fan_in ** -0.5

    def gain(shape):
        return 1.0 + 0.05 * jax.random.normal(next(ks), shape, f32)

    def small(shape, s=0.02):
        return s * jax.random.normal(next(ks), shape, f32)

    nA, nB = N_A_LAYERS, N_B_LAYERS
    return {
        "x": jax.random.normal(next(ks), (BATCH, SEQ, D_MODEL), f32),
        "c": jax.random.normal(next(ks), (BATCH, D_MODEL), f32),
        "rel_bias": small((NUM_BUCKETS, A_HEADS), 0.2),
        "a_w_in": w((nA, D_MODEL, A_IN), D_MODEL),
        "a_g_cq": gain((nA, Q_RANK)),
        "a_g_ckv": gain((nA, KV_RANK)),
        "a_w_uq": w((nA, Q_RANK, A_HEADS * A_HEAD_DIM), Q_RANK),
        "a_w_uk": w((nA, A_HEADS, A_HEAD_DIM, KV_RANK), A_HEAD_DIM),
        "a_w_uv": w((nA, A_HEADS, KV_RANK, A_V_DIM), KV_RANK),
        "a_w_qi": w((nA, Q_RANK, IDX_HEADS * IDX_DIM), Q_RANK),
        "a_w_o": w((nA, A_HEADS * A_V_DIM, D_MODEL), A_HEADS * A_V_DIM),
        "b_w_in": w((nB, D_MODEL, B_IN), D_MODEL),
        "b_w_g2": w((nB, G_RANK, G_KD), G_RANK),
        "b_b_g": small((nB, G_KD)),
        "b_g_norm": gain((nB, G_VD)),
        "b_w_o": w((nB, G_VD, D_MODEL), G_VD),
        "ada_w": w((DEPTH, D_MODEL, 6 * D_MODEL), D_MODEL),
        "ada_b": small((DEPTH, 6 * D_MODEL)),
        "g_mix": gain((DEPTH, D_MODEL)),
        "g_ffn": gain((DEPTH, D_MODEL)),
        "f_w_in": w((DEPTH, D_MODEL, 2 * D_FF), D_MODEL),
        "f_conv_w": w((DEPTH, CONV_WIDTH, D_FF), CONV_WIDTH),
        "f_conv_b": small((DEPTH, D_FF)),
        "f_w_out": w((DEPTH, D_FF, D_MODEL), D_FF),
        "g_final": gain((D_MODEL,)),
    }


def reference(x, c, rel_bias, a_w_in, a_g_cq, a_g_ckv, a_w_uq, a_w_uk, a_w_uv, a_w_qi, a_w_o,
              b_w_in, b_w_g2, b_b_g, b_g_norm, b_w_o, ada_w, ada_b, g_mix, g_ffn,
              f_w_in, f_conv_w, f_conv_b, f_w_out, g_final):
    cond = jax.nn.silu(c)
    for i in range(DEPTH):
        mod = cond @ ada_w[i] + ada_b[i]
        sh_m, sc_m, gt_m, sh_f, sc_f, gt_f = jnp.split(mod[:, None, :], 6, axis=-1)
        h = rmsnorm(x, g_mix[i]) * (1 + sc_m) + sh_m
        j = i // N_MIXERS
        if i % N_MIXERS == 0:
            y = dsa_mixer(h, rel_bias, a_w_in[j], a_g_cq[j], a_g_ckv[j], a_w_uq[j],
                          a_w_uk[j], a_w_uv[j], a_w_qi[j], a_w_o[j])
        else:
            y = gla_mixer(h, b_w_in[j], b_w_g2[j], b_b_g[j], b_g_norm[j], b_w_o[j])
        x = x + gt_m * y
        h = rmsnorm(x, g_ffn[i]) * (1 + sc_f) + sh_f
        x = x + gt_f * conv_ffn(h, f_w_in[i], f_conv_w[i], f_conv_b[i], f_w_out[i])
    return rmsnorm(x, g_final)
```

```python
import math
from contextlib import ExitStack
import numpy as np
from concourse.bass_utils import run_bass_kernel_spmd
import concourse.bass as bass
import concourse.mybir as mybir

F32 = mybir.dt.float32
BF16 = mybir.dt.bfloat16
ALU = mybir.AluOpType
AF = mybir.ActivationFunctionType
AX = mybir.AxisListType


class _Op:
    __slots__ = ("eng", "meth", "args", "kw", "deps", "need", "semval", "dma", "dsem", "dval", "dprev")

    def __init__(self, eng, meth, args, kw, dma):
        self.eng, self.meth, self.args, self.kw, self.dma = eng, meth, args, kw, dma
        self.deps = []
        self.need = False
        self.semval = 0
        self.dsem = None
        self.dval = 0
        self.dprev = None


def _region(ap):
    t = ap.tensor
    name = t.name
    dims = ap.ap
    off = int(ap.offset)
    if str(ap.space) == "DRAM":
        ext = 1
        for st, ct in dims:
            ext += (ct - 1) * abs(st)
        return name, (0, 1, off, off + ext)
    if str(ap.space) == "PSUM":
        return name, (0, 128, 0, 1 << 30)
    pstep = dims[0][0]
    if pstep == 0:
        pstep = 1 << 40
    plo = off // pstep if pstep < (1 << 40) else 0
    flo = off - plo * pstep if pstep < (1 << 40) else off
    ext = 1
    for st, ct in dims[1:]:
        ext += (ct - 1) * abs(st)
    return name, (plo, plo + dims[0][1], flo, flo + ext)


def _ovl(a, b):
    return a[0] < b[1] and b[0] < a[1] and a[2] < b[3] and b[2] < a[3]


def _cov(a, b):
    return a[0] <= b[0] and a[1] >= b[1] and a[2] <= b[2] and a[3] >= b[3]


class Sched:
    NDS = 8

    def __init__(self, nc):
        self.nc = nc
        self.engs = {"pe": nc.tensor, "act": nc.scalar, "dve": nc.vector, "pool": nc.gpsimd, "sp": nc.sync}
        self.ops = []
        self.wr = {}
        self.rd = {}
        self.ndma = {k: 0 for k in self.engs}
        self.dma_hist = {k: [] for k in self.engs}

    def _track(self, op, reads, writes):
        psr = [ap for ap in reads if str(ap.space) == "PSUM"]
        if psr:
            reads = [ap for ap in reads if str(ap.space) != "PSUM"]
            writes = list(writes) + psr
        deps = set()
        for ap in reads:
            name, rg = _region(ap)
            for (r2, o2) in self.wr.get(name, ()):
                if _ovl(rg, r2):
                    deps.add(o2)
        for ap in writes:
            name, rg = _region(ap)
            for (r2, o2) in self.wr.get(name, ()):
                if _ovl(rg, r2):
                    deps.add(o2)
            for (r2, o2) in self.rd.get(name, ()):
                if _ovl(rg, r2):
                    deps.add(o2)
        for ap in writes:
            name, rg = _region(ap)
            self.wr[name] = [(r2, o2) for (r2, o2) in self.wr.get(name, ()) if not _cov(rg, r2)]
            self.rd[name] = [(r2, o2) for (r2, o2) in self.rd.get(name, ()) if not _cov(rg, r2)]
            self.wr[name].append((rg, op))
        for ap in reads:
            name, rg = _region(ap)
            lst = self.rd.setdefault(name, [])
            lst[:] = [(r2, o2) for (r2, o2) in lst if not (o2.eng == op.eng and not o2.dma and not op.dma and _cov(rg, r2))]
            lst.append((rg, op))
        for d in deps:
            if d is op:
                continue
            op.deps.append(d)
            d.need = True

    def op(self, eng, meth, *args, r=(), w=(), **kw):
        o = _Op(eng, meth, args, kw, False)
        self._track(o, list(r), list(w))
        self.ops.append(o)
        return o

    def dma(self, eng, out, in_, **kw):
        o = _Op(eng, "dma_start", (), dict(out=out, in_=in_, **kw), True)
        o.need = True
        self._track(o, [in_], [out])
        h = self.dma_hist[eng]
        j = len(h)
        o.dsem = j % self.NDS
        o.dval = 16 * (j // self.NDS + 1)
        if j >= self.NDS:
            o.dprev = h[j - self.NDS]
        h.append(o)
        self.ops.append(o)
        return o

    def mm(self, out, lhsT, rhs, start=True, stop=True, **kw):
        return self.op("pe", "matmul", out, lhsT, rhs, start=start, stop=stop, r=[lhsT, rhs], w=[out], **kw)

    def transpose(self, out, in_, ident):
        return self.op("pe", "transpose", out, in_, ident, r=[in_, ident], w=[out])

    def act(self, out, in_, func, bias=None, scale=None, accum_out=None, eng="act"):
        kw = {}
        r = [in_]
        w = [out]
        if bias is not None:
            kw["bias"] = bias
            if not isinstance(bias, (int, float)):
                r.append(bias)
        if scale is not None:
            kw["scale"] = scale
            if not isinstance(scale, (int, float)):
                r.append(scale)
        if accum_out is not None:
            kw["accum_out"] = accum_out
            w.append(accum_out)
        return self.op(eng, "activation", out, in_, func, r=r, w=w, **kw)

    def tt(self, eng, out, in0, in1, op):
        return self.op(eng, "tensor_tensor", out, in0, in1, op, r=[in0, in1], w=[out])

    def ts(self, eng, out, in0, s1, s2, op0, op1=None, accum_out=None):
        r = [in0] + [s for s in (s1, s2) if s is not None and not isinstance(s, (int, float))]
        w = [out] + ([accum_out] if accum_out is not None else [])
        kw = {}
        if op1 is not None:
            kw["op1"] = op1
        if accum_out is not None:
            kw["accum_out"] = accum_out
        return self.op(eng, "tensor_scalar", out, in0, s1, s2, op0, r=r, w=w, **kw)

    def stt(self, eng, out, in0, scalar, in1, op0, op1):
        r = [in0, in1] + ([scalar] if not isinstance(scalar, (int, float)) else [])
        return self.op(eng, "scalar_tensor_tensor", out, in0, scalar, in1, op0, op1, r=r, w=[out])

    def copy(self, eng, out, in_):
        if eng == "act":
            return self.op("act", "copy", out, in_, r=[in_], w=[out])
        return self.op(eng, "tensor_copy", out, in_, r=[in_], w=[out])

    def memset(self, eng, ap, val):
        return self.op(eng, "memset", ap, val, r=[], w=[ap])

    def barrier(self):
        last = {}
        for o in self.ops:
            if not o.dma and o.meth is not None:
                last[o.eng] = o
        dm = []
        for k, h in self.dma_hist.items():
            dm += h[-self.NDS:]
        for e in self.engs:
            b = _Op(e, None, (), {}, False)
            for k, o in last.items():
                if k != e:
                    b.deps.append(o)
                    o.need = True
            b.deps += dm
            self.ops.append(b)
        self.wr.clear()
        self.rd.clear()

    def emit(self):
        nc = self.nc
        sems = {k: nc.alloc_semaphore("s_" + k) for k in self.engs}
        dsems = {}
        for k in self.engs:
            if self.dma_hist[k]:
                dsems[k] = [nc.alloc_semaphore(f"d_{k}{i}") for i in range(self.NDS)]
        cnt = {k: 0 for k in self.engs}
        for o in self.ops:
            if o.dma:
                continue
            if o.need:
                cnt[o.eng] += 1
                o.semval = cnt[o.eng]
        waited = {k: {} for k in self.engs}

        def wait(eng, sem, val):
            key = sem.num
            if waited[eng].get(key, 0) >= val:
                return
            waited[eng][key] = val
            self.engs[eng].wait_ge(sem, val)

        nwait = 0
        for o in self.ops:
            e = self.engs[o.eng]
            if o.dma and o.dprev is not None:
                wait(o.eng, dsems[o.eng][o.dprev.dsem], o.dprev.dval)
            for d in o.deps:
                if d.dma:
                    wait(o.eng, dsems[d.eng][d.dsem], d.dval)
                else:
                    if d.eng == o.eng and o.eng == "pe":
                        continue
                    wait(o.eng, sems[d.eng], d.semval)
            if o.meth is None:
                continue
            try:
                ins = getattr(e, o.meth)(*o.args, **o.kw)
            except BaseException as ex:
                print("EMIT FAIL", o.eng, o.meth, [str(a)[:80] for a in o.args], {k: str(v)[:60] for k, v in o.kw.items()})
                raise
            if o.dma:
                ins.then_inc(dsems[o.eng][o.dsem], 16)
            elif o.need:
                ins.then_inc(sems[o.eng], 1)
        for k, h in self.dma_hist.items():
            for o in h[-self.NDS:]:
                wait("sp", dsems[k][o.dsem], o.dval)
        for k in self.engs:
            if k != "sp" and cnt[k] > 0:
                wait("sp", sems[k], cnt[k])
        return len(self.ops)


D = 1024
DFF = 2816
NFC = DFF // 128
EPS = 1e-6
NIT = 16


def t5_bucket_np(n):
    n = np.maximum(n, 0)
    exact = 16
    lr = np.log(np.maximum(n, 1).astype(np.float32) / np.float32(exact)) / np.float32(math.log(128 / exact))
    large = exact + (lr.astype(np.float32) * np.float32(32 - exact)).astype(np.int32)
    return np.where(n < exact, n, np.minimum(large, 31))


def make_consts():
    c = np.zeros((128, 1024), np.float32)
    i = np.arange(128)
    c[:, 0:128] = np.eye(128)
    c[:, 128:256] = (i[None, :] >= i[:, None])
    c[:, 256:384] = np.where(i[None, :] <= i[:, None], 0.0, -1e30)
    c[:, 384:512] = np.where(i[:, None] <= i[None, :], -1.0 / 16, 0.0)
    c[:, 512:640] = np.where(i[:, None] > i[None, :], -1.0 / 16, 0.0)
    m = np.arange(384)
    d = m - 127
    bk = t5_bucket_np(d)
    oh = np.zeros((32, 384), np.float32)
    for mm_ in range(383):
        if d[mm_] >= 0:
            oh[bk[mm_], mm_] = 1.0
    c[0:32, 640:1024] = oh
    return c


def fbc(ap, reps):
    dims = ap.ap
    return bass.AP(ap.tensor, ap.offset, [list(dims[0]), [0, reps]] + [list(d) for d in dims[1:]])


def build(T=2048, NB=2, nst=5, dbg=False, lvl=9):
    nc = bass.Bass("TRN2", target_bir_lowering=False)
    S = Sched(nc)
    NBLK = T // 128
    NG = T // 512
    NSEL = min(256, T // 4)
    NB0 = NSEL // 128

    def din(name, shape):
        return nc.dram_tensor(name, shape, F32, kind="ExternalInput").ap()

    x = din("x", [NB, T, D]); cin = din("c", [NB, D]); rel_bias = din("rel_bias", [32, 8])
    a_w_in = din("a_w_in", [D, 584]); a_g_cq = din("a_g_cq", [2, 128]); a_g_ckv = din("a_g_ckv", [2, 128])
    a_w_uq = din("a_w_uq", [256, 1024]); a_w_uk = din("a_w_uk", [8, 128, 256]); a_w_uv = din("a_w_uv", [8, 256, 128])
    a_w_qi = din("a_w_qi", [256, 512]); a_w_o = din("a_w_o", [1024, 1024])
    b_w_in = din("b_w_in", [D, 3088]); b_w_g2 = din("b_w_g2", [16, 512]); b_b_g = din("b_b_g", [1, 512])
    b_g_norm = din("b_g_norm", [8, 128]); b_w_o = din("b_w_o", [1024, 1024])
    ada_w = din("ada_w", [2, D, 6144]); ada_b = din("ada_b", [96, 128])
    g_mix = din("g_mix", [16, 128]); g_ffn = din("g_ffn", [16, 128])
    f_w_in = din("f_w_in", [2, D, 2 * DFF]); f_conv_w = din("f_conv_w", [2, 66, 128]); f_conv_b = din("f_conv_b", [44, 128])
    f_w_out = din("f_w_out", [2, DFF, D]); g_final = din("g_final", [8, 128])
    consts = din("consts", [128, 1024])
    out = nc.dram_tensor("out", [NB, T, D], F32, kind="ExternalOutput").ap()
    douts = [nc.dram_tensor(f"out{i}", [NB, T, D], F32, kind="ExternalOutput").ap() for i in range(1, 5)] if dbg else []
    bvp = nc.dram_tensor("bvp", [128, 8 * 384], F32).ap()
    dbg_outs = {}

    def sb(name, shape, dt=F32):
        return nc.alloc_sbuf_tensor(name, shape, dt).ap()

    cst = sb("cst", [128, 1024])
    ident = cst[:, 0:128]
    cmask = cst[:, 256:384]
    TRI = cst[:, 384:512]
    TRIR = cst[:, 512:640]
    OH = cst[0:32, 640:1024]
    ident_bf = sb("ident_bf", [128, 128], BF16)
    triT_bf = sb("triT_bf", [128, 128], BF16)
    ones_bf = sb("ones_bf", [128, 128], BF16)
    onesD_bf = sb("onesD_bf", [128, 128], BF16)
    ones256_bf = sb("ones256_bf", [128, 128], BF16)
    eps_t = sb("eps_t", [128, 1])
    xT = sb("xT", [128, 8, T])
    vecs = sb("vecs", [128, 320])
    modT = sb("modT", [128, 2, 48, NB])
    gsT = sb("gsT", [128, 2, NB, 2, 8])
    condT = sb("condT", [128, 8, NB], BF16)
    P = [nc.alloc_psum_tensor(f"P{i}", [128, 512], F32).ap() for i in range(7)]
    P.append(nc.alloc_psum_tensor("P7", [128, 512], F32).ap())
    rot = {"ev": 0, "uid": 0}

    def uid():
        rot["uid"] += 1
        return "_%d" % rot["uid"]

    def ev_eng():
        rot["ev"] ^= 1
        return "act" if rot["ev"] else "dve"

    def evac(out_ap, in_ap):
        S.copy(ev_eng(), out_ap, in_ap)

    S.dma("sp", cst, consts)
    S.copy("dve", ident_bf, ident)
    S.copy("dve", triT_bf, cst[:, 128:256])
    S.memset("dve", ones_bf, 1.0)
    S.memset("dve", onesD_bf, 1.0 / D)
    S.memset("dve", ones256_bf, 1.0 / 256)
    S.memset("dve", eps_t, EPS)

    V_C, V_ADAB, V_GMIX, V_GFFN, V_GFIN, V_GCQ, V_GCKV, V_GNORM, V_CW, V_CB = 0, 16, 112, 128, 144, 152, 154, 156, 164, 296
    vecs2 = sb("vecs2", [128, 64])
    with ExitStack() as es:
        stg = es.enter_context(nc.sbuf_tensor("stg", [128, 128], F32)).ap()
        stg2 = es.enter_context(nc.sbuf_tensor("stg2", [128, 128], F32)).ap()
        stgs = [stg, stg2]
        k = [0]

        def load_T(src, R, dst):
            st = stgs[k[0] % 2]
            k[0] += 1
            S.dma("sp", st[0:R, :], src)
            S.transpose(P[0][:, 0:R], st[0:R, :], ident[0:R, 0:R])
            S.copy("dve", dst, P[0][:, 0:R])

        load_T(cin.rearrange("b (k p) -> (b k) p", p=128), NB * 8, vecs[:, V_C:V_C + NB * 8])
        load_T(ada_b, 96, vecs[:, V_ADAB:V_ADAB + 96])
        load_T(g_mix, 16, vecs[:, V_GMIX:V_GMIX + 16])
        load_T(g_ffn, 16, vecs[:, V_GFFN:V_GFFN + 16])
        load_T(g_final, 8, vecs[:, V_GFIN:V_GFIN + 8])
        load_T(a_g_cq, 2, vecs[:, V_GCQ:V_GCQ + 2])
        load_T(a_g_ckv, 2, vecs[:, V_GCKV:V_GCKV + 2])
        load_T(b_g_norm, 8, vecs[:, V_GNORM:V_GNORM + 8])
        load_T(f_conv_w[0], 66, vecs[:, V_CW:V_CW + 66])
        load_T(f_conv_w[1], 66, vecs[:, V_CW + 66:V_CW + 132])
        load_T(f_conv_b, 44, vecs2[:, 0:44])
        for b in range(NB):
            S.act(condT[:, :, b], vecs[:, V_C + b * 8:V_C + b * 8 + 8], AF.Silu)

        rb = es.enter_context(nc.sbuf_tensor("rb", [32, 8], F32)).ap()
        rb31 = es.enter_context(nc.sbuf_tensor("rb31", [32, 8], F32)).ap()
        rbl = es.enter_context(nc.sbuf_tensor("rbl", [32, 8, 128], F32)).ap()
        bvrep = es.enter_context(nc.sbuf_tensor("bvrep", [128, 8 * 384], F32)).ap()
        S.dma("sp", rb, rel_bias)
        S.dma("sp", rb31, bass.AP(rel_bias.tensor, 31 * 8, [[0, 32], [1, 8]]))
        S.tt("dve", rb, rb, rb31, ALU.subtract)
        for h in range(8):
            S.copy("dve", rbl[:, h, :], bass.AP(rb.tensor, rb[:, h:h + 1].offset, [list(rb.ap[0]), [0, 128]]))
            S.mm(P[1][:, 0:384], rbl[:, h, :], OH)
            S.copy("act", bvrep[:, h * 384:(h + 1) * 384], P[1][:, 0:384])
        S.dma("sp", bvp, bvrep)

        adaw = [es.enter_context(nc.sbuf_tensor(f"adaw{i}", [128, 8, 512], BF16)).ap() for i in range(2)]
        for l in range(2):
            for j in range(12):
                slot = adaw[(l * 12 + j) % 2]
                S.dma("pool", slot, ada_w[l][:, j * 512:(j + 1) * 512].rearrange("(k p) n -> p k n", p=128))
                for jj in range(4):
                    cj = j * 4 + jj
                    for kc in range(8):
                        S.mm(P[2][:, cj * NB:(cj + 1) * NB], slot[:, kc, jj * 128:(jj + 1) * 128], condT[:, kc, :],
                             start=(kc == 0), stop=(kc == 7))
            ab = vecs[:, V_ADAB + l * 48:V_ADAB + (l + 1) * 48]
            abb = bass.AP(ab.tensor, ab.offset, [list(ab.ap[0]), [1, 48], [0, NB]])
            S.tt("dve", modT[:, l], P[2][:, 0:48 * NB].rearrange("p (j b) -> p j b", b=NB), abb, ALU.add)
            for b in range(NB):
                S.stt("dve", gsT[:, l, b, 0, :], modT[:, l, 8:16, b], 1.0, vecs[:, V_GMIX + l * 8:V_GMIX + l * 8 + 8], ALU.add, ALU.mult)
                S.stt("dve", gsT[:, l, b, 1, :], modT[:, l, 32:40, b], 1.0, vecs[:, V_GFFN + l * 8:V_GFFN + l * 8 + 8], ALU.add, ALU.mult)
        S.barrier()

    def norm_to(hT, t0, W, scale_ap, shift_ap, sqb, rt, tmpb, bank=0):
        for kc in range(8):
            sq = sqb[kc % 2]
            S.act(sq[:, 0:W], xT[:, kc, t0:t0 + W], AF.Square)
            S.mm(P[bank][:, 0:W], onesD_bf, sq[:, 0:W], start=(kc == 0), stop=(kc == 7))
        S.act(rt[:, 0:W], P[bank][:, 0:W], AF.Sqrt, bias=eps_t)
        S.op("dve", "reciprocal", rt[:, 0:W], rt[:, 0:W], r=[rt[:, 0:W]], w=[rt[:, 0:W]])
        for kc in range(8):
            tm = tmpb[kc % 2]
            S.stt("dve", tm[:, 0:W], xT[:, kc, t0:t0 + W], scale_ap[:, kc:kc + 1], rt[:, 0:W], ALU.mult, ALU.mult)
            if shift_ap is None:
                S.copy("act", hT[:, kc, 0:W], tm[:, 0:W])
            else:
                S.act(hT[:, kc, 0:W], tm[:, 0:W], AF.Identity, bias=shift_ap[:, kc:kc + 1])

    def load_x(b):
        with ExitStack() as es:
            xin = [es.enter_context(nc.sbuf_tensor(f"xin{i}_{b}", [128, D], F32)).ap() for i in range(2)]
            for tt_ in range(NBLK):
                xi = xin[tt_ % 2]
                S.dma("sp", xi, x[b, tt_ * 128:(tt_ + 1) * 128, :])
                for q4 in range(2):
                    pb = P[1 + (tt_ * 2 + q4) % 4]
                    for kk in range(4):
                        kc = q4 * 4 + kk
                        S.transpose(pb[:, kk * 128:(kk + 1) * 128], xi[:, kc * 128:(kc + 1) * 128], ident)
                    evac(xT[:, q4 * 4:q4 * 4 + 4, tt_ * 128:(tt_ + 1) * 128], pb.rearrange("p (k t) -> p k t", k=4))
            S.barrier()

    def ffn_phase(b, l):
        with ExitStack() as es:
            A = lambda name, shape, dt=F32: es.enter_context(nc.sbuf_tensor(name + uid(), shape, dt)).ap()
            w_in = A("f_win", [128, 8, 2 * DFF], BF16)
            gT = A("f_gT", [128, NFC, 512], BF16)
            hT = A("f_hT", [128, 8, 512], BF16)
            NRING = 3
            ring = [A(f"f_ring{i}", [128, 1024], BF16) for i in range(NRING)]
            usb = [A("f_usb0", [128, 512], F32)] * 2
            tcb = [A(f"f_tc{i}", [128, 512], F32) for i in range(2)]
            sqb = [A("f_sq0", [128, 512], BF16)] * 2
            tmpb = [A(f"f_tmp{i}", [128, 512], F32) for i in range(2)]
            glb = tmpb
            rt = tcb[1]
            carry = A("f_carry", [128, NFC, 2])
            CB = DFF // 2
            for c0 in (0, DFF, CB, DFF + CB):
                for kc in range(8):
                    S.dma("pool", w_in[:, kc, c0:c0 + CB], f_w_in[l][kc * 128:(kc + 1) * 128, c0:c0 + CB])
            cw = vecs[:, V_CW + l * 66:V_CW + (l + 1) * 66]
            cb = vecs2[:, l * NFC:(l + 1) * NFC]
            gt = modT[:, l, 40:48, b]
            for g in range(NG):
                t0 = g * 512
                norm_to(hT, t0, 512, gsT[:, l, b, 1, :], modT[:, l, 24:32, b], sqb, rt, tmpb)
                for fc in range(NRING):
                    S.dma("pool", ring[fc], f_w_out[l][fc * 128:(fc + 1) * 128, :])
                for fc in range(NFC):
                    ups, vps = P[1 + (fc % 3) * 2], P[2 + (fc % 3) * 2]
                    for kc in range(8):
                        S.mm(ups, w_in[:, kc, fc * 128:(fc + 1) * 128], hT[:, kc, :], start=(kc == 0), stop=(kc == 7))
                    for kc in range(8):
                        S.mm(vps, w_in[:, kc, DFF + fc * 128:DFF + (fc + 1) * 128], hT[:, kc, :], start=(kc == 0), stop=(kc == 7))
                    t1 = usb[fc % 2]
                    tc = tcb[fc % 2]
                    gl = glb[fc % 2]
                    w2, w1, w0 = cw[:, 2 * NFC + fc:2 * NFC + fc + 1], cw[:, NFC + fc:NFC + fc + 1], cw[:, fc:fc + 1]
                    S.act(t1[:, 0:512], ups, AF.Identity, scale=w2, bias=cb[:, fc:fc + 1])
                    S.stt("dve", tc[:, 1:512], ups[:, 0:511], w1, t1[:, 1:512], ALU.mult, ALU.add)
                    S.stt("dve", tc[:, 2:512], ups[:, 0:510], w0, tc[:, 2:512], ALU.mult, ALU.add)
                    if g == 0:
                        S.copy("dve", tc[:, 0:1], t1[:, 0:1])
                    else:
                        S.stt("dve", tc[:, 0:1], carry[:, fc, 1:2], w1, t1[:, 0:1], ALU.mult, ALU.add)
                        S.stt("dve", tc[:, 0:2], carry[:, fc, 0:2], w0, tc[:, 0:2], ALU.mult, ALU.add)
                    if g < NG - 1:
                        S.copy("dve", carry[:, fc, :], ups[:, 510:512])
                    S.act(gl, tc, AF.Gelu)
                    S.tt("dve", gT[:, fc, :], gl, vps, ALU.mult)
                for fc in range(NFC):
                    if fc >= NRING:
                        S.dma("pool", ring[fc % NRING], f_w_out[l][fc * 128:(fc + 1) * 128, :])
                    for ncx in range(8):
                        S.mm(P[ncx], ring[fc % NRING][:, ncx * 128:(ncx + 1) * 128], gT[:, fc, :], start=(fc == 0), stop=(fc == NFC - 1))
                for ncx in range(8):
                    S.stt("dve", xT[:, ncx, t0:t0 + 512], P[ncx], gt[:, ncx:ncx + 1], xT[:, ncx, t0:t0 + 512], ALU.mult, ALU.add)
            S.barrier()

    def dsa_phase(b, l):
        with ExitStack() as es:
            A = lambda name, shape, dt=F32: es.enter_context(nc.sbuf_tensor(name + uid(), shape, dt)).ap()
            w_in = A("a_win", [128, 8, 584], BF16)
            w_k2 = A("a_wk2", [128, 8, 128], BF16)
            w_uq = A("a_wuq", [128, 2, 1024], BF16)
            w_uk = A("a_wuk", [128, 8, 256], BF16)
            w_uv = A("a_wuv", [128, 8, 2, 128], BF16)
            w_qi = A("a_wqi", [128, 2, 512], BF16)
            w_o = A("a_wo", [128, 8, 1024], BF16)
            ckvT = A("a_ckvT", [128, 2, T], BF16)
            ckv_tok = A("a_ckvtok", [128, NBLK, 256], BF16)
            kidxT = A("a_kidxT", [128, T], BF16)
            hT = A("a_hT", [128, 8, 512], BF16)
            sqb = [A(f"a_sq{i}", [128, 512], BF16) for i in range(2)]
            tmpb = [A(f"a_tmp{i}", [128, 512], F32) for i in range(2)]
            gsc = A("a_gsc", [128, 2048])
            rt = gsc[:, 1536:2048]
            raw = gsc[:, 0:1024].rearrange("p (a b) -> p a b", a=2)
            sq2 = A("a_sq2", [128, 2, 512], BF16)
            rt2 = gsc[:, 1024:1536]
            cqT = A("a_cqT", [128, 2, 512], BF16)
            widx = A("a_widx", [128, 4, 8])
            qTb = A("a_qTb", [128, 8, 128], BF16)
            qlatT = [A(f"a_qlatT{i}", [128, 2, 1024], BF16) for i in range(2)]
            qidxT = A("a_qidxT", [128, 4, 128], BF16)
            scoreb = [hT.bitcast(F32).rearrange("p a b -> p (a b)")[:, 0:T], gsc[:, 0:T]]
            Rb = sqb
            Dg = sq2.rearrange("p a (b c) -> p (a b) c", c=128)
            sel = A("a_sel", [128, T], BF16)
            selT = [A(f"a_selT{i}", [128, NBLK, 128], BF16) for i in range(2)]
            bis = A("a_bis", [128, 8])
            Eb = [A(f"a_E{i}", [128, 512], BF16) for i in range(2)]
            Ssb = A("a_Ssb", [128, 512])
            PTb = [A(f"a_PT{i}", [128, 512], BF16) for i in range(2)]
            rec = Ssb
            olatT = A("a_olatT", [128, 2, 1024], BF16)
            oTb = A("a_oTb", [128, 8, 128], BF16)
            BT = A("a_BT", [128, 2, 1024])
            for dl in range(2):
                src = bass.AP(bvp.tensor, 128 * dl + 127, [[8 * 384 - 1, 128], [384, 8], [1, 128]])
                S.dma("sp", BT[:, dl, :].rearrange("p (h q) -> p h q", h=8), src)

            S.dma("pool", w_in, a_w_in.rearrange("(k p) n -> p k n", p=128))
            for hf in range(2):
                S.dma("pool", w_k2[:, :, hf * 64:(hf + 1) * 64], a_w_in[:, 512:576].rearrange("(k p) n -> p k n", p=128))
            S.dma("pool", w_uq, a_w_uq.rearrange("(k p) n -> p k n", p=128))
            S.dma("pool", w_uk, a_w_uk.rearrange("h d c -> d h c"))
            S.dma("pool", w_uv, a_w_uv.rearrange("h (cc c) v -> c h cc v", cc=2))
            S.dma("pool", w_qi, a_w_qi.rearrange("(k p) n -> p k n", p=128))
            S.dma("pool", w_o, a_w_o.rearrange("(k p) n -> p k n", p=128))
            gcq = vecs[:, V_GCQ:V_GCQ + 2]
            gckv = vecs[:, V_GCKV:V_GCKV + 2]
            gt = modT[:, l, 16:24, b]
            st = {"nE": 0}

            def lat_norm(col0, gvec, dst_fn):
                for oc in range(2):
                    ps = P[5 + oc]
                    for kc in range(8):
                        S.mm(ps, w_in[:, kc, col0 + oc * 128:col0 + (oc + 1) * 128], hT[:, kc, :], start=(kc == 0), stop=(kc == 7))
                    S.copy("dve", raw[:, oc, :], ps)
                    S.act(sq2[:, oc, :], ps, AF.Square)
                for oc in range(2):
                    S.mm(P[7], ones256_bf, sq2[:, oc, :], start=(oc == 0), stop=(oc == 1))
                S.act(rt2, P[7], AF.Sqrt, bias=eps_t)
                S.op("dve", "reciprocal", rt2, rt2, r=[rt2], w=[rt2])
                for oc in range(2):
                    S.stt("dve", dst_fn(oc), raw[:, oc, :], gvec[:, oc:oc + 1], rt2, ALU.mult, ALU.mult)

            def group_level(g):
                t0 = g * 512
                norm_to(hT, t0, 512, gsT[:, l, b, 0, :], modT[:, l, 0:8, b], sqb, rt, tmpb, bank=7)
                lat_norm(0, gcq, lambda oc: cqT[:, oc, :])
                lat_norm(256, gckv, lambda oc: ckvT[:, oc, t0:t0 + 512])
                for kc in range(8):
                    S.mm(P[5], w_k2[:, kc, :], hT[:, kc, :], start=(kc == 0), stop=(kc == 7))
                S.copy("act", kidxT[:, t0:t0 + 512], P[5])
                for j in range(4):
                    for kc in range(8):
                        S.mm(P[6][:, j * 8:(j + 1) * 8], hT[:, kc, j * 128:(j + 1) * 128], w_in[:, kc, 576:584], start=(kc == 0), stop=(kc == 7))
                S.copy("dve", widx.rearrange("p a b -> p (a b)"), P[6][:, 0:32])
                for j in range(4):
                    for cc in range(2):
                        S.mm(P[5 + j // 2][:, ((j % 2) * 2 + cc) * 128:((j % 2) * 2 + cc + 1) * 128], ckvT[:, cc, t0 + j * 128:t0 + (j + 1) * 128], ident_bf)
                for j2 in range(2):
                    S.copy("act", ckv_tok[:, 4 * g + 2 * j2:4 * g + 2 * j2 + 2, :].rearrange("p a c -> p (a c)"), P[5 + j2])

            def sel_part(g, j):
                n = 4 * g + j
                Tk = (n + 1) * 128
                tq = n * 128
                tql = j * 128
                ql = qlatT[n % 2]
                sT = selT[n % 2]
                score = scoreb[n % 2]
                for h in range(8):
                    ps = P[5 + h // 4]
                    for cc in range(2):
                        S.mm(ps[:, (h % 4) * 128:(h % 4 + 1) * 128], w_uq[:, cc, h * 128:(h + 1) * 128], cqT[:, cc, tql:tql + 128],
                             start=(cc == 0), stop=(cc == 1))
                for q2 in range(2):
                    S.copy("act", qTb[:, q2 * 4:(q2 + 1) * 4, :].rearrange("p a c -> p (a c)"), P[5 + q2])
                for cc in range(2):
                    for h in range(8):
                        ps = P[5 + h // 4]
                        S.mm(ps[:, (h % 4) * 128:(h % 4 + 1) * 128], w_uk[:, h, cc * 128:(cc + 1) * 128], qTb[:, h, :])
                    for q2 in range(2):
                        S.act(ql[:, cc, q2 * 512:(q2 + 1) * 512], P[5 + q2], AF.Identity, scale=128 ** -0.5)
                for pr in range(4):
                    for cc in range(2):
                        S.mm(P[7][:, pr * 128:(pr + 1) * 128], w_qi[:, cc, pr * 128:(pr + 1) * 128], cqT[:, cc, tql:tql + 128],
                             start=(cc == 0), stop=(cc == 1))
                S.copy("act", qidxT.rearrange("p a c -> p (a c)"), P[7])
                for h in range(8):
                    S.ts("pool", Dg[:, h, :], ident_bf, widx[:, j, h:h + 1], None, ALU.mult)
                items = [(kk, h) for kk in range((Tk + 511) // 512) for h in range(8)]

                def idx_mm(i):
                    kk, h = items[i]
                    w = min(512, Tk - kk * 512)
                    pr, hf = h // 2, h % 2
                    S.mm(P[5 + i % 2][:, 0:w], qidxT[64 * hf:64 * hf + 64, pr, :], kidxT[64 * hf:64 * hf + 64, kk * 512:kk * 512 + w])

                idx_mm(0)
                for i in range(len(items)):
                    kk, h = items[i]
                    w = min(512, Tk - kk * 512)
                    if i + 1 < len(items):
                        idx_mm(i + 1)
                    R = Rb[i % 2]
                    S.act(R[:, 0:w], P[5 + i % 2][:, 0:w], AF.Relu)
                    S.mm(P[7][:, 0:w], Dg[:, h, :], R[:, 0:w], start=(h == 0), stop=(h == 7))
                    if h == 7:
                        S.copy("act", score[:, kk * 512:kk * 512 + w], P[7][:, 0:w])
                sv = score[:, 0:Tk]
                if n >= NB0:
                    S.op("dve", "tensor_reduce", bis[:, 0:1], sv, AX.X, ALU.max, r=[sv], w=[bis[:, 0:1]])
                    S.op("dve", "tensor_reduce", bis[:, 1:2], sv, AX.X, ALU.min, r=[sv], w=[bis[:, 1:2]])
                    S.tt("dve", bis[:, 2:3], bis[:, 0:1], bis[:, 1:2], ALU.subtract)
                S.tt("dve", score[:, tq:tq + 128], score[:, tq:tq + 128], cmask, ALU.add)

            def bis_gen(g, j):
                n = 4 * g + j
                Tk = (n + 1) * 128
                score = scoreb[n % 2]
                sv = score[:, 0:Tk]
                if n >= NB0:
                    S.ts("dve", bis[:, 3:4], bis[:, 2:3], 0.5, bis[:, 1:2], ALU.mult, op1=ALU.add)
                    for it in range(1, NIT + 1):
                        f = 2.0 ** -it
                        S.ts("dve", sel[:, 0:Tk], sv, bis[:, 3:4], None, ALU.is_ge, op1=ALU.add, accum_out=bis[:, 4:5])
                        if it < NIT:
                            S.ts("dve", bis[:, 5:6], bis[:, 4:5], float(NSEL), f, ALU.is_ge, op1=ALU.mult)
                            S.stt("dve", bis[:, 3:4], bis[:, 5:6], bis[:, 2:3], bis[:, 3:4], ALU.mult, ALU.add)
                            S.stt("dve", bis[:, 3:4], bis[:, 2:3], -f / 2, bis[:, 3:4], ALU.mult, ALU.add)
                        else:
                            S.ts("dve", bis[:, 5:6], bis[:, 4:5], float(NSEL), -1.0, ALU.is_ge, op1=ALU.add)
                            S.ts("dve", bis[:, 5:6], bis[:, 5:6], f, None, ALU.mult)
                            S.stt("dve", bis[:, 1:2], bis[:, 5:6], bis[:, 2:3], bis[:, 3:4], ALU.mult, ALU.add)
                        yield
                    S.ts("dve", sel[:, 0:Tk], sv, bis[:, 1:2], None, ALU.is_ge)
                else:
                    S.ts("dve", sel[:, 0:Tk], sv, -1e29, None, ALU.is_ge)
                yield

            def att_part(g, j, gen):
                n = 4 * g + j
                tq = n * 128
                ql = qlatT[n % 2]
                sT = selT[n % 2]
                for k0 in range(0, n + 1, 4):
                    cntc = min(4, n + 1 - k0)
                    pb = P[5 + (k0 // 4) % 2]
                    for kc in range(k0, k0 + cntc):
                        S.mm(pb[:, (kc - k0) * 128:(kc - k0 + 1) * 128], sel[:, kc * 128:(kc + 1) * 128], ident_bf)
                    S.copy("act", sT[:, k0:k0 + cntc, :].rearrange("p a c -> p (a c)"), pb[:, 0:cntc * 128])
                steps = [(hh, kc) for hh in range(2) for kc in range(n + 1)]
                base = st["nE"]
                st["nE"] += len(steps)

                def qk(i):
                    hh, kc = steps[i]
                    Sps = P[1 + (base + i) % 2]
                    for cc in range(2):
                        S.mm(Sps, ckvT[:, cc, kc * 128:(kc + 1) * 128], ql[:, cc, hh * 512:(hh + 1) * 512], start=(cc == 0), stop=(cc == 1))

                qk(0)
                for i in range(len(steps)):
                    hh, kc = steps[i]
                    if i + 1 < len(steps):
                        qk(i + 1)
                    Sps = P[1 + (base + i) % 2]
                    E = Eb[(base + i) % 2]
                    PTt = PTb[(base + i) % 2]
                    dl = n - kc
                    if dl <= 1:
                        S.tt("dve", Ssb, Sps, BT[:, dl, hh * 512:(hh + 1) * 512], ALU.add)
                        S.act(E, Ssb, AF.Exp)
                    else:
                        S.act(E, Sps, AF.Exp)
                    S.tt("pool", PTt.rearrange("p (h q) -> p h q", h=4), E.rearrange("p (h q) -> p h q", h=4), fbc(sT[:, kc, :], 4), ALU.mult)
                    for cc in range(2):
                        S.mm(P[3 + cc], ckv_tok[:, kc, cc * 128:(cc + 1) * 128], PTt, start=(kc == 0), stop=(kc == n))
                    S.mm(P[0], ones_bf, PTt, start=(kc == 0), stop=(kc == n))
                    if kc == n:
                        S.act(rec, P[0], AF.Ln)
                        S.act(rec, rec, AF.Exp, scale=-1.0)
                        for cc in range(2):
                            S.tt("dve", olatT[:, cc, hh * 512:(hh + 1) * 512], P[3 + cc], rec, ALU.mult)
                    if gen is not None:
                        next(gen, None)
                if gen is not None:
                    for _ in gen:
                        pass
                for h in range(8):
                    ps = P[1 + h // 4]
                    for cc in range(2):
                        S.mm(ps[:, (h % 4) * 128:(h % 4 + 1) * 128], w_uv[:, h, cc, :], olatT[:, cc, h * 128:(h + 1) * 128], start=(cc == 0), stop=(cc == 1))
                for q2 in range(2):
                    S.copy("act", oTb[:, q2 * 4:(q2 + 1) * 4, :].rearrange("p a c -> p (a c)"), P[1 + q2])
                for ncx in range(8):
                    ps = P[3 + ncx // 4]
                    for h in range(8):
                        S.mm(ps[:, (ncx % 4) * 128:(ncx % 4 + 1) * 128], w_o[:, h, ncx * 128:(ncx + 1) * 128], oTb[:, h, :], start=(h == 0), stop=(h == 7))
                for ncx in range(8):
                    ps = P[3 + ncx // 4]
                    S.stt("dve", xT[:, ncx, tq:tq + 128], ps[:, (ncx % 4) * 128:(ncx % 4 + 1) * 128], gt[:, ncx:ncx + 1], xT[:, ncx, tq:tq + 128], ALU.mult, ALU.add)

            prev = None
            for g in range(NG):
                group_level(g)
                for j in range(4):
                    sel_part(g, j)
                    gen = bis_gen(g, j)
                    if prev is None:
                        for _ in gen:
                            pass
                    else:
                        att_part(prev[0], prev[1], gen)
                    prev = (g, j)
            att_part(prev[0], prev[1], None)
            S.barrier()

    def gla_phase(b, l):
        with ExitStack() as es:
            A = lambda name, shape, dt=F32: es.enter_context(nc.sbuf_tensor(name + uid(), shape, dt)).ap()
            w_in = A("b_win", [128, 8, 3088], BF16)
            w_o = A("b_wo", [128, 8, 1024], BF16)
            w_g2 = A("b_wg2", [32, 512])
            hT = A("b_hT", [128, 8, 512], BF16)
            sqb = [A(f"b_sq{i}", [128, 512], BF16) for i in range(2)]
            tmpb = [A(f"b_tmp{i}", [128, 512], F32) for i in range(2)]
            rt = A("b_rt", [128, 512])
            qraw = A("b_qraw", [128, 4, 512])
            kraw = A("b_kraw", [128, 4, 512])
            rs = A("b_rs", [128, 8, 512], BF16)
            rtmp = tmpb
            glr = A("b_glr", [32, 512])
            ogT = A("b_ogT", [128, 8, 512], BF16)
            L = A("b_L", [128, 512])
            E1 = A("b_E1", [128, 512])
            E2 = A("b_E2", [128, 512])
            E3 = L
            qt = A("b_qt", [128, 4, 128], BF16)
            kt = A("b_kt", [128, 4, 128], BF16)
            kh = A("b_kh", [128, 512], BF16)
            vb = A("b_vb", [128, 1024], BF16)
            AT = A("b_AT", [128, 4, 128], BF16)
            St = A("b_S", [128, 4, 256])
            Sbf = A("b_Sbf", [128, 4, 256], BF16)
            on = E2.bitcast(BF16)
            ssq = A("b_ssq", [128, 8])
            junk = rt[:, 0:256]

            S.dma("pool", w_in, b_w_in.rearrange("(k p) n -> p k n", p=128))
            S.dma("pool", w_o, b_w_o.rearrange("(k p) n -> p k n", p=128))
            S.dma("sp", w_g2[0:16, :], b_w_g2)
            S.dma("sp", w_g2[16:17, :], b_b_g)
            S.memset("dve", St, 0.0)
            S.memset("dve", Sbf, 0.0)
            S.memset("dve", glr, 1.0)
            gn = vecs[:, V_GNORM:V_GNORM + 8]
            gt = modT[:, l, 16:24, b]
            for g in range(NG):
                t0 = g * 512
                norm_to(hT, t0, 512, gsT[:, l, b, 0, :], modT[:, l, 0:8, b], sqb, rt, tmpb)
                for h in range(4):
                    for (dst, c0) in ((qraw, 0), (kraw, 512)):
                        ps = P[1 + (h % 2)] if dst is qraw else P[3 + (h % 2)]
                        for kc in range(8):
                            S.mm(ps, w_in[:, kc, c0 + h * 128:c0 + (h + 1) * 128], hT[:, kc, :], start=(kc == 0), stop=(kc == 7))
                        evac(dst[:, h, :], ps)
                for c8 in range(8):
                    ps = P[5 + c8 % 2]
                    for kc in range(8):
                        S.mm(ps, w_in[:, kc, 2048 + c8 * 128:2048 + (c8 + 1) * 128], hT[:, kc, :], start=(kc == 0), stop=(kc == 7))
                    S.act(rtmp[c8 % 2], ps, AF.Silu)
                    S.ts("dve", rs[:, c8, :], rtmp[c8 % 2], gn[:, c8:c8 + 1], None, ALU.mult)
                for kc in range(8):
                    S.mm(P[0][0:16, :], w_in[:, kc, 3072:3088], hT[:, kc, :], start=(kc == 0), stop=(kc == 7))
                S.copy("dve", glr[0:16, :], P[0][0:16, :])
                for j in range(4):
                    tl = j * 128
                    tq = t0 + tl
                    for kc in range(8):
                        S.mm(P[1], hT[:, kc, tl:tl + 128], w_in[:, kc, 512:1024], start=(kc == 0), stop=(kc == 7))
                    for vh in range(2):
                        for kc in range(8):
                            S.mm(P[2 + vh], hT[:, kc, tl:tl + 128], w_in[:, kc, 1024 + vh * 512:1024 + (vh + 1) * 512], start=(kc == 0), stop=(kc == 7))
                    S.mm(P[4], glr[0:17, tl:tl + 128], w_g2[0:17, :])
                    S.act(L, P[4], AF.Exp, scale=-1.0)
                    S.act(L, L, AF.Ln, bias=1.0)
                    for h in range(4):
                        S.mm(P[5][:, h * 128:(h + 1) * 128], L[:, h * 128:(h + 1) * 128], TRI)
                    S.mm(P[6], TRIR, L)
                    S.act(E1, P[5], AF.Exp)
                    S.act(E2, P[5], AF.Exp, scale=-1.0)
                    S.act(E3, P[6], AF.Exp)
                    S.stt("dve", qt, qraw[:, :, tl:tl + 128], 128 ** -0.5, E1.rearrange("p (h t) -> p h t", h=4), ALU.mult, ALU.mult)
                    S.tt("dve", kt, kraw[:, :, tl:tl + 128], E2.rearrange("p (h t) -> p h t", h=4), ALU.mult)
                    S.tt("dve", kh, P[1], E3, ALU.mult)
                    S.copy("act", vb[:, 0:512], P[2])
                    S.copy("act", vb[:, 512:1024], P[3])
                    for h in range(4):
                        S.mm(P[4][:, h * 128:(h + 1) * 128], kt[:, h, :], qt[:, h, :])
                    S.tt("dve", AT, P[4].rearrange("p (h t) -> p h t", h=4), fbc(triT_bf, 4), ALU.mult)
                    for h in range(4):
                        po = P[1 + h // 2][:, (h % 2) * 256:(h % 2 + 1) * 256]
                        S.mm(po, qt[:, h, :], Sbf[:, h, :], start=True, stop=False)
                        S.mm(po, AT[:, h, :], vb[:, h * 256:(h + 1) * 256], start=False, stop=True)
                    for h in range(4):
                        pn = P[5 + h // 2][:, (h % 2) * 256:(h % 2 + 1) * 256]
                        S.mm(pn, kh[:, h * 128:(h + 1) * 128], vb[:, h * 256:(h + 1) * 256])
                    for h in range(4):
                        pn = P[5 + h // 2][:, (h % 2) * 256:(h % 2 + 1) * 256]
                        S.stt("dve", St[:, h, :], St[:, h, :], E1[:, h * 128 + 127:h * 128 + 128], pn, ALU.mult, ALU.add)
                        S.copy("act", Sbf[:, h, :], St[:, h, :])
                    S.memset("dve", ssq[:, 0:4], 0.0)
                    for h in range(4):
                        po = P[1 + h // 2][:, (h % 2) * 256:(h % 2 + 1) * 256]
                        S.act(junk, po, AF.Square, accum_out=ssq[:, h:h + 1])
                    S.act(ssq[:, 4:8], ssq[:, 0:4], AF.Sqrt, bias=eps_t, scale=1.0 / 256)
                    S.op("dve", "reciprocal", ssq[:, 4:8], ssq[:, 4:8], r=[ssq[:, 4:8]], w=[ssq[:, 4:8]])
                    for h in range(4):
                        po = P[1 + h // 2][:, (h % 2) * 256:(h % 2 + 1) * 256]
                        S.ts("dve", on[:, h * 256:(h + 1) * 256], po, ssq[:, 4 + h:5 + h], None, ALU.mult)
                    for c8 in range(8):
                        S.mm(P[0 if c8 < 4 else 7][:, (c8 % 4) * 128:(c8 % 4 + 1) * 128], on[:, c8 * 128:(c8 + 1) * 128], ident_bf)
                    for q2 in range(2):
                        S.tt("dve", ogT[:, q2 * 4:(q2 + 1) * 4, tl:tl + 128], P[0 if q2 == 0 else 7].rearrange("p (c t) -> p c t", c=4), rs[:, q2 * 4:(q2 + 1) * 4, tl:tl + 128], ALU.mult)
                for half in range(2):
                    for n4 in range(4):
                        ncx = half * 4 + n4
                        for c8 in range(8):
                            S.mm(P[1 + n4], w_o[:, c8, ncx * 128:(ncx + 1) * 128], ogT[:, c8, :], start=(c8 == 0), stop=(c8 == 7))
                    for n4 in range(4):
                        ncx = half * 4 + n4
                        S.stt("dve", xT[:, ncx, t0:t0 + 512], P[1 + n4], gt[:, ncx:ncx + 1], xT[:, ncx, t0:t0 + 512], ALU.mult, ALU.add)
            S.barrier()

    def final_out(b, out=out):
        with ExitStack() as es:
            A = lambda name, shape, dt=F32: es.enter_context(nc.sbuf_tensor(name + uid(), shape, dt)).ap()
            hN = A("o_hN", [128, 8, 512])
            sqb = [A(f"o_sq{i}", [128, 512], BF16) for i in range(2)]
            tmpb = [A(f"o_tmp{i}", [128, 512], F32) for i in range(2)]
            rt = A("o_rt", [128, 512])
            ob = [A(f"o_ob{i}", [128, D]) for i in range(2)]
            gf = vecs[:, V_GFIN:V_GFIN + 8]
            for g in range(NG):
                t0 = g * 512
                norm_to(hN, t0, 512, gf, None, sqb, rt, tmpb)
                for j in range(4):
                    o_ = ob[j % 2]
                    for q4 in range(2):
                        pb = P[1 + (j * 2 + q4) % 4]
                        for kk in range(4):
                            kc = q4 * 4 + kk
                            S.transpose(pb[:, kk * 128:(kk + 1) * 128], hN[:, kc, j * 128:(j + 1) * 128], ident)
                        evac(o_[:, q4 * 512:(q4 + 1) * 512], pb)
                    S.dma("sp", out[b, t0 + j * 128:t0 + (j + 1) * 128, :], o_)
            S.barrier()

    for b in range(NB):
        load_x(b)
        if dbg:
            final_out(b, douts[0])
        if nst >= 2:
            dsa_phase(b, 0)
            if dbg:
                final_out(b, douts[1])
        if nst >= 3:
            ffn_phase(b, 0)
            if dbg:
                final_out(b, douts[2])
        if nst >= 4:
            gla_phase(b, 1)
            if dbg:
                final_out(b, douts[3])
        if nst >= 5:
            ffn_phase(b, 1)
        final_out(b)
    n = S.emit()
    return nc, n


def prep_inputs(inp, b0, NB, T):
    f = lambda a: np.ascontiguousarray(np.asarray(a, dtype=np.float32))
    m = {
        "x": f(inp["x"][b0:b0 + NB, :T]), "c": f(inp["c"][b0:b0 + NB]), "rel_bias": f(inp["rel_bias"]),
        "a_w_in": f(inp["a_w_in"][0]), "a_g_cq": f(inp["a_g_cq"][0].reshape(2, 128)), "a_g_ckv": f(inp["a_g_ckv"][0].reshape(2, 128)),
        "a_w_uq": f(inp["a_w_uq"][0]), "a_w_uk": f(inp["a_w_uk"][0]), "a_w_uv": f(inp["a_w_uv"][0]),
        "a_w_qi": f(inp["a_w_qi"][0]), "a_w_o": f(inp["a_w_o"][0]),
        "b_w_in": f(inp["b_w_in"][0]), "b_w_g2": f(inp["b_w_g2"][0]), "b_b_g": f(inp["b_b_g"][0].reshape(1, 512)),
        "b_g_norm": f(inp["b_g_norm"][0].reshape(8, 128)), "b_w_o": f(inp["b_w_o"][0]),
        "ada_w": f(inp["ada_w"]), "ada_b": f(inp["ada_b"].reshape(96, 128)),
        "g_mix": f(inp["g_mix"].reshape(16, 128)), "g_ffn": f(inp["g_ffn"].reshape(16, 128)),
        "f_w_in": f(inp["f_w_in"]), "f_conv_w": f(inp["f_conv_w"].reshape(2, 66, 128)), "f_conv_b": f(inp["f_conv_b"].reshape(44, 128)),
        "f_w_out": f(inp["f_w_out"]), "g_final": f(inp["g_final"].reshape(8, 128)),
        "consts": make_consts(),
    }
    return m


_T = 2048
_NB = 2
_NCORES = 8


def kernel(**inputs):
    inp = {k: np.asarray(v) for k, v in inputs.items()}
    nc, _ = build(T=_T, NB=_NB)
    in_maps = [prep_inputs(inp, core * _NB, _NB, _T) for core in range(_NCORES)]
    res = run_bass_kernel_spmd(nc, in_maps, core_ids=list(range(_NCORES)))
    outs = [np.asarray(r["out"], dtype=np.float32) for r in res.results]
    return np.concatenate(outs, axis=0)
```

```python
import math
from contextlib import ExitStack
import numpy as np
from concourse.bass_utils import run_bass_kernel_spmd
import concourse.bass as bass
import concourse.mybir as mybir

F32 = mybir.dt.float32
BF16 = mybir.dt.bfloat16
ALU = mybir.AluOpType
AF = mybir.ActivationFunctionType
AX = mybir.AxisListType


class _Op:
    __slots__ = ("eng", "meth", "args", "kw", "deps", "need", "semval", "dma", "dsem", "dval", "dprev")

    def __init__(self, eng, meth, args, kw, dma):
        self.eng, self.meth, self.args, self.kw, self.dma = eng, meth, args, kw, dma
        self.deps = []
        self.need = False
        self.semval = 0
        self.dsem = None
        self.dval = 0
        self.dprev = None


def _region(ap):
    t = ap.tensor
    name = t.name
    dims = ap.ap
    off = int(ap.offset)
    if str(ap.space) == "DRAM":
        ext = 1
        for st, ct in dims:
            ext += (ct - 1) * abs(st)
        return name, (0, 1, off, off + ext)
    if str(ap.space) == "PSUM":
        return name, (0, 128, 0, 1 << 30)
    pstep = dims[0][0]
    if pstep == 0:
        pstep = 1 << 40
    plo = off // pstep if pstep < (1 << 40) else 0
    flo = off - plo * pstep if pstep < (1 << 40) else off
    ext = 1
    for st, ct in dims[1:]:
        ext += (ct - 1) * abs(st)
    return name, (plo, plo + dims[0][1], flo, flo + ext)


def _ovl(a, b):
    return a[0] < b[1] and b[0] < a[1] and a[2] < b[3] and b[2] < a[3]


def _cov(a, b):
    return a[0] <= b[0] and a[1] >= b[1] and a[2] <= b[2] and a[3] >= b[3]


class Sched:
    NDS = 8

    def __init__(self, nc):
        self.nc = nc
        self.engs = {"pe": nc.tensor, "act": nc.scalar, "dve": nc.vector, "pool": nc.gpsimd, "sp": nc.sync}
        self.ops = []
        self.wr = {}
        self.rd = {}
        self.ndma = {k: 0 for k in self.engs}
        self.dma_hist = {k: [] for k in self.engs}

    def _track(self, op, reads, writes):
        psr = [ap for ap in reads if str(ap.space) == "PSUM"]
        if psr:
            reads = [ap for ap in reads if str(ap.space) != "PSUM"]
            writes = list(writes) + psr
        deps = set()
        for ap in reads:
            name, rg = _region(ap)
            for (r2, o2) in self.wr.get(name, ()):
                if _ovl(rg, r2):
                    deps.add(o2)
        for ap in writes:
            name, rg = _region(ap)
            for (r2, o2) in self.wr.get(name, ()):
                if _ovl(rg, r2):
                    deps.add(o2)
            for (r2, o2) in self.rd.get(name, ()):
                if _ovl(rg, r2):
                    deps.add(o2)
        for ap in writes:
            name, rg = _region(ap)
            self.wr[name] = [(r2, o2) for (r2, o2) in self.wr.get(name, ()) if not _cov(rg, r2)]
            self.rd[name] = [(r2, o2) for (r2, o2) in self.rd.get(name, ()) if not _cov(rg, r2)]
            self.wr[name].append((rg, op))
        for ap in reads:
            name, rg = _region(ap)
            lst = self.rd.setdefault(name, [])
            lst[:] = [(r2, o2) for (r2, o2) in lst if not (o2.eng == op.eng and not o2.dma and not op.dma and _cov(rg, r2))]
            lst.append((rg, op))
        for d in deps:
            if d is op:
                continue
            op.deps.append(d)
            d.need = True

    def op(self, eng, meth, *args, r=(), w=(), **kw):
        o = _Op(eng, meth, args, kw, False)
        self._track(o, list(r), list(w))
        self.ops.append(o)
        return o

    def dma(self, eng, out, in_, **kw):
        o = _Op(eng, "dma_start", (), dict(out=out, in_=in_, **kw), True)
        o.need = True
        self._track(o, [in_], [out])
        h = self.dma_hist[eng]
        j = len(h)
        o.dsem = j % self.NDS
        o.dval = 16 * (j // self.NDS + 1)
        if j >= self.NDS:
            o.dprev = h[j - self.NDS]
        h.append(o)
        self.ops.append(o)
        return o

    def mm(self, out, lhsT, rhs, start=True, stop=True, **kw):
        return self.op("pe", "matmul", out, lhsT, rhs, start=start, stop=stop, r=[lhsT, rhs], w=[out], **kw)

    def transpose(self, out, in_, ident):
        return self.op("pe", "transpose", out, in_, ident, r=[in_, ident], w=[out])

    def act(self, out, in_, func, bias=None, scale=None, accum_out=None, eng="act"):
        kw = {}
        r = [in_]
        w = [out]
        if bias is not None:
            kw["bias"] = bias
            if not isinstance(bias, (int, float)):
                r.append(bias)
        if scale is not None:
            kw["scale"] = scale
            if not isinstance(scale, (int, float)):
                r.append(scale)
        if accum_out is not None:
            kw["accum_out"] = accum_out
            w.append(accum_out)
        return self.op(eng, "activation", out, in_, func, r=r, w=w, **kw)

    def tt(self, eng, out, in0, in1, op):
        return self.op(eng, "tensor_tensor", out, in0, in1, op, r=[in0, in1], w=[out])

    def ts(self, eng, out, in0, s1, s2, op0, op1=None, accum_out=None):
        r = [in0] + [s for s in (s1, s2) if s is not None and not isinstance(s, (int, float))]
        w = [out] + ([accum_out] if accum_out is not None else [])
        kw = {}
        if op1 is not None:
            kw["op1"] = op1
        if accum_out is not None:
            kw["accum_out"] = accum_out
        return self.op(eng, "tensor_scalar", out, in0, s1, s2, op0, r=r, w=w, **kw)

    def stt(self, eng, out, in0, scalar, in1, op0, op1):
        r = [in0, in1] + ([scalar] if not isinstance(scalar, (int, float)) else [])
        return self.op(eng, "scalar_tensor_tensor", out, in0, scalar, in1, op0, op1, r=r, w=[out])

    def copy(self, eng, out, in_):
        if eng == "act":
            return self.op("act", "copy", out, in_, r=[in_], w=[out])
        return self.op(eng, "tensor_copy", out, in_, r=[in_], w=[out])

    def memset(self, eng, ap, val):
        return self.op(eng, "memset", ap, val, r=[], w=[ap])

    def barrier(self):
        last = {}
        for o in self.ops:
            if not o.dma and o.meth is not None:
                last[o.eng] = o
        dm = []
        for k, h in self.dma_hist.items():
            dm += h[-self.NDS:]
        for e in self.engs:
            b = _Op(e, None, (), {}, False)
            for k, o in last.items():
                if k != e:
                    b.deps.append(o)
                    o.need = True
            b.deps += dm
            self.ops.append(b)
        self.wr.clear()
        self.rd.clear()

    def emit(self):
        nc = self.nc
        sems = {k: nc.alloc_semaphore("s_" + k) for k in self.engs}
        dsems = {}
        for k in self.engs:
            if self.dma_hist[k]:
                dsems[k] = [nc.alloc_semaphore(f"d_{k}{i}") for i in range(self.NDS)]
        cnt = {k: 0 for k in self.engs}
        for o in self.ops:
            if o.dma:
                continue
            if o.need:
                cnt[o.eng] += 1
                o.semval = cnt[o.eng]
        waited = {k: {} for k in self.engs}

        def wait(eng, sem, val):
            key = sem.num
            if waited[eng].get(key, 0) >= val:
                return
            waited[eng][key] = val
            self.engs[eng].wait_ge(sem, val)

        nwait = 0
        for o in self.ops:
            e = self.engs[o.eng]
            if o.dma and o.dprev is not None:
                wait(o.eng, dsems[o.eng][o.dprev.dsem], o.dprev.dval)
            for d in o.deps:
                if d.dma:
                    wait(o.eng, dsems[d.eng][d.dsem], d.dval)
                else:
                    if d.eng == o.eng and o.eng == "pe":
                        continue
                    wait(o.eng, sems[d.eng], d.semval)
            if o.meth is None:
                continue
            try:
                ins = getattr(e, o.meth)(*o.args, **o.kw)
            except BaseException as ex:
                print("EMIT FAIL", o.eng, o.meth, [str(a)[:80] for a in o.args], {k: str(v)[:60] for k, v in o.kw.items()})
                raise
            if o.dma:
                ins.then_inc(dsems[o.eng][o.dsem], 16)
            elif o.need:
                ins.then_inc(sems[o.eng], 1)
        for k, h in self.dma_hist.items():
            for o in h[-self.NDS:]:
                wait("sp", dsems[k][o.dsem], o.dval)
        for k in self.engs:
            if k != "sp" and cnt[k] > 0:
                wait("sp", sems[k], cnt[k])
        return len(self.ops)


D = 1024
DFF = 2816
NFC = DFF // 128
EPS = 1e-6
NIT = 16


def t5_bucket_np(n):
    n = np.maximum(n, 0)
    exact = 16
    lr = np.log(np.maximum(n, 1).astype(np.float32) / np.float32(exact)) / np.float32(math.log(128 / exact))
    large = exact + (lr.astype(np.float32) * np.float32(32 - exact)).astype(np.int32)
    return np.where(n < exact, n, np.minimum(large, 31))


def make_consts():
    c = np.zeros((128, 1024), np.float32)
    i = np.arange(128)
    c[:, 0:128] = np.eye(128)
    c[:, 128:256] = (i[None, :] >= i[:, None])
    c[:, 256:384] = np.where(i[None, :] <= i[:, None], 0.0, -1e30)
    c[:, 384:512] = np.where(i[:, None] <= i[None, :], -1.0 / 16, 0.0)
    c[:, 512:640] = np.where(i[:, None] > i[None, :], -1.0 / 16, 0.0)
    m = np.arange(384)
    d = m - 127
    bk = t5_bucket_np(d)
    oh = np.zeros((32, 384), np.float32)
    for mm_ in range(383):
        if d[mm_] >= 0:
            oh[bk[mm_], mm_] = 1.0
    c[0:32, 640:1024] = oh
    return c


def fbc(ap, reps):
    dims = ap.ap
    return bass.AP(ap.tensor, ap.offset, [list(dims[0]), [0, reps]] + [list(d) for d in dims[1:]])


def build(T=2048, NB=2, nst=5, dbg=False, lvl=9):
    nc = bass.Bass("TRN2", target_bir_lowering=False)
    S = Sched(nc)
    NBLK = T // 128
    NG = T // 512
    NSEL = min(256, T // 4)
    NB0 = NSEL // 128

    def din(name, shape):
        return nc.dram_tensor(name, shape, F32, kind="ExternalInput").ap()

    x = din("x", [NB, T, D]); cin = din("c", [NB, D]); rel_bias = din("rel_bias", [32, 8])
    a_w_in = din("a_w_in", [D, 584]); a_g_cq = din("a_g_cq", [2, 128]); a_g_ckv = din("a_g_ckv", [2, 128])
    a_w_uq = din("a_w_uq", [256, 1024]); a_w_uk = din("a_w_uk", [8, 128, 256]); a_w_uv = din("a_w_uv", [8, 256, 128])
    a_w_qi = din("a_w_qi", [256, 512]); a_w_o = din("a_w_o", [1024, 1024])
    b_w_in = din("b_w_in", [D, 3088]); b_w_g2 = din("b_w_g2", [16, 512]); b_b_g = din("b_b_g", [1, 512])
    b_g_norm = din("b_g_norm", [8, 128]); b_w_o = din("b_w_o", [1024, 1024])
    ada_w = din("ada_w", [2, D, 6144]); ada_b = din("ada_b", [96, 128])
    g_mix = din("g_mix", [16, 128]); g_ffn = din("g_ffn", [16, 128])
    f_w_in = din("f_w_in", [2, D, 2 * DFF]); f_conv_w = din("f_conv_w", [2, 66, 128]); f_conv_b = din("f_conv_b", [44, 128])
    f_w_out = din("f_w_out", [2, DFF, D]); g_final = din("g_final", [8, 128])
    consts = din("consts", [128, 1024])
    out = nc.dram_tensor("out", [NB, T, D], F32, kind="ExternalOutput").ap()
    douts = [nc.dram_tensor(f"out{i}", [NB, T, D], F32, kind="ExternalOutput").ap() for i in range(1, 5)] if dbg else []
    bvp = nc.dram_tensor("bvp", [128, 8 * 384], F32).ap()
    dbg_outs = {}

    def sb(name, shape, dt=F32):
        return nc.alloc_sbuf_tensor(name, shape, dt).ap()

    cst = sb("cst", [128, 1024])
    ident = cst[:, 0:128]
    cmask = cst[:, 256:384]
    TRI = cst[:, 384:512]
    TRIR = cst[:, 512:640]
    OH = cst[0:32, 640:1024]
    ident_bf = sb("ident_bf", [128, 128], BF16)
    triT_bf = sb("triT_bf", [128, 128], BF16)
    ones_bf = sb("ones_bf", [128, 128], BF16)
    onesD_bf = sb("onesD_bf", [128, 128], BF16)
    ones256_bf = sb("ones256_bf", [128, 128], BF16)
    eps_t = sb("eps_t", [128, 1])
    xT = sb("xT", [128, 8, T])
    vecs = sb("vecs", [128, 320])
    modT = sb("modT", [128, 2, 48, NB])
    gsT = sb("gsT", [128, 2, NB, 2, 8])
    condT = sb("condT", [128, 8, NB], BF16)
    P = [nc.alloc_psum_tensor(f"P{i}", [128, 512], F32).ap() for i in range(7)]
    P.append(nc.alloc_psum_tensor("P7", [128, 512], F32).ap())
    rot = {"ev": 0, "uid": 0}

    def uid():
        rot["uid"] += 1
        return "_%d" % rot["uid"]

    def ev_eng():
        rot["ev"] ^= 1
        return "act" if rot["ev"] else "dve"

    def evac(out_ap, in_ap):
        S.copy(ev_eng(), out_ap, in_ap)

    S.dma("sp", cst, consts)
    S.copy("dve", ident_bf, ident)
    S.copy("dve", triT_bf, cst[:, 128:256])
    S.memset("dve", ones_bf, 1.0)
    S.memset("dve", onesD_bf, 1.0 / D)
    S.memset("dve", ones256_bf, 1.0 / 256)
    S.memset("dve", eps_t, EPS)

    V_C, V_ADAB, V_GMIX, V_GFFN, V_GFIN, V_GCQ, V_GCKV, V_GNORM, V_CW, V_CB = 0, 16, 112, 128, 144, 152, 154, 156, 164, 296
    vecs2 = sb("vecs2", [128, 64])
    with ExitStack() as es:
        stg = es.enter_context(nc.sbuf_tensor("stg", [128, 128], F32)).ap()
        stg2 = es.enter_context(nc.sbuf_tensor("stg2", [128, 128], F32)).ap()
        stgs = [stg, stg2]
        k = [0]

        def load_T(src, R, dst):
            st = stgs[k[0] % 2]
            k[0] += 1
            S.dma("sp", st[0:R, :], src)
            S.transpose(P[0][:, 0:R], st[0:R, :], ident[0:R, 0:R])
            S.copy("dve", dst, P[0][:, 0:R])

        load_T(cin.rearrange("b (k p) -> (b k) p", p=128), NB * 8, vecs[:, V_C:V_C + NB * 8])
        load_T(ada_b, 96, vecs[:, V_ADAB:V_ADAB + 96])
        load_T(g_mix, 16, vecs[:, V_GMIX:V_GMIX + 16])
        load_T(g_ffn, 16, vecs[:, V_GFFN:V_GFFN + 16])
        load_T(g_final, 8, vecs[:, V_GFIN:V_GFIN + 8])
        load_T(a_g_cq, 2, vecs[:, V_GCQ:V_GCQ + 2])
        load_T(a_g_ckv, 2, vecs[:, V_GCKV:V_GCKV + 2])
        load_T(b_g_norm, 8, vecs[:, V_GNORM:V_GNORM + 8])
        load_T(f_conv_w[0], 66, vecs[:, V_CW:V_CW + 66])
        load_T(f_conv_w[1], 66, vecs[:, V_CW + 66:V_CW + 132])
        load_T(f_conv_b, 44, vecs2[:, 0:44])
        for b in range(NB):
            S.act(condT[:, :, b], vecs[:, V_C + b * 8:V_C + b * 8 + 8], AF.Silu)

        rb = es.enter_context(nc.sbuf_tensor("rb", [32, 8], F32)).ap()
        rb31 = es.enter_context(nc.sbuf_tensor("rb31", [32, 8], F32)).ap()
        rbl = es.enter_context(nc.sbuf_tensor("rbl", [32, 8, 128], F32)).ap()
        bvrep = es.enter_context(nc.sbuf_tensor("bvrep", [128, 8 * 384], F32)).ap()
        S.dma("sp", rb, rel_bias)
        S.dma("sp", rb31, bass.AP(rel_bias.tensor, 31 * 8, [[0, 32], [1, 8]]))
        S.tt("dve", rb, rb, rb31, ALU.subtract)
        for h in range(8):
            S.copy("dve", rbl[:, h, :], bass.AP(rb.tensor, rb[:, h:h + 1].offset, [list(rb.ap[0]), [0, 128]]))
            S.mm(P[1][:, 0:384], rbl[:, h, :], OH)
            S.copy("act", bvrep[:, h * 384:(h + 1) * 384], P[1][:, 0:384])
        S.dma("sp", bvp, bvrep)

        adaw = [es.enter_context(nc.sbuf_tensor(f"adaw{i}", [128, 8, 512], BF16)).ap() for i in range(4)]
        for l in range(2):
            for j in range(12):
                slot = adaw[(l * 12 + j) % 4]
                S.dma("pool", slot, ada_w[l][:, j * 512:(j + 1) * 512].rearrange("(k p) n -> p k n", p=128))
                for jj in range(4):
                    cj = j * 4 + jj
                    for kc in range(8):
                        S.mm(P[2][:, cj * NB:(cj + 1) * NB], slot[:, kc, jj * 128:(jj + 1) * 128], condT[:, kc, :],
                             start=(kc == 0), stop=(kc == 7))
            ab = vecs[:, V_ADAB + l * 48:V_ADAB + (l + 1) * 48]
            abb = bass.AP(ab.tensor, ab.offset, [list(ab.ap[0]), [1, 48], [0, NB]])
            S.tt("dve", modT[:, l], P[2][:, 0:48 * NB].rearrange("p (j b) -> p j b", b=NB), abb, ALU.add)
            for b in range(NB):
                S.stt("dve", gsT[:, l, b, 0, :], modT[:, l, 8:16, b], 1.0, vecs[:, V_GMIX + l * 8:V_GMIX + l * 8 + 8], ALU.add, ALU.mult)
                S.stt("dve", gsT[:, l, b, 1, :], modT[:, l, 32:40, b], 1.0, vecs[:, V_GFFN + l * 8:V_GFFN + l * 8 + 8], ALU.add, ALU.mult)
        S.barrier()

    def norm_to(hT, t0, W, scale_ap, shift_ap, sqb, rt, tmpb, bank=0):
        for kc in range(8):
            sq = sqb[kc % 2]
            S.act(sq[:, 0:W], xT[:, kc, t0:t0 + W], AF.Square)
            S.mm(P[bank][:, 0:W], onesD_bf, sq[:, 0:W], start=(kc == 0), stop=(kc == 7))
        S.act(rt[:, 0:W], P[bank][:, 0:W], AF.Sqrt, bias=eps_t)
        S.op("dve", "reciprocal", rt[:, 0:W], rt[:, 0:W], r=[rt[:, 0:W]], w=[rt[:, 0:W]])
        for kc in range(8):
            tm = tmpb[kc % 2]
            S.stt("dve", tm[:, 0:W], xT[:, kc, t0:t0 + W], scale_ap[:, kc:kc + 1], rt[:, 0:W], ALU.mult, ALU.mult)
            if shift_ap is None:
                S.copy("act", hT[:, kc, 0:W], tm[:, 0:W])
            else:
                S.act(hT[:, kc, 0:W], tm[:, 0:W], AF.Identity, bias=shift_ap[:, kc:kc + 1])

    def load_x(b):
        with ExitStack() as es:
            xin = [es.enter_context(nc.sbuf_tensor(f"xin{i}_{b}", [128, D], F32)).ap() for i in range(4)]
            for tt_ in range(NBLK):
                xi = xin[tt_ % 4]
                S.dma("sp", xi, x[b, tt_ * 128:(tt_ + 1) * 128, :])
                for q4 in range(2):
                    pb = P[1 + (tt_ * 2 + q4) % 4]
                    for kk in range(4):
                        kc = q4 * 4 + kk
                        S.transpose(pb[:, kk * 128:(kk + 1) * 128], xi[:, kc * 128:(kc + 1) * 128], ident)
                    evac(xT[:, q4 * 4:q4 * 4 + 4, tt_ * 128:(tt_ + 1) * 128], pb.rearrange("p (k t) -> p k t", k=4))
            S.barrier()

    def ffn_phase(b, l):
        with ExitStack() as es:
            A = lambda name, shape, dt=F32: es.enter_context(nc.sbuf_tensor(name + uid(), shape, dt)).ap()
            w_in = A("f_win", [128, 8, 2 * DFF], BF16)
            gT = A("f_gT", [128, NFC, 512], BF16)
            hT = A("f_hT", [128, 8, 512], BF16)
            NRING = 3
            ring = [A(f"f_ring{i}", [128, 1024], BF16) for i in range(NRING)]
            usb = [A("f_usb0", [128, 512], F32)] * 2
            tcb = [A(f"f_tc{i}", [128, 512], F32) for i in range(2)]
            sqb = [A("f_sq0", [128, 512], BF16)] * 2
            tmpb = [A(f"f_tmp{i}", [128, 512], F32) for i in range(2)]
            glb = tmpb
            rt = tcb[1]
            carry = A("f_carry", [128, NFC, 2])
            CB = DFF // 2
            for c0 in (0, DFF, CB, DFF + CB):
                for kc in range(8):
                    S.dma("pool", w_in[:, kc, c0:c0 + CB], f_w_in[l][kc * 128:(kc + 1) * 128, c0:c0 + CB])
            cw = vecs[:, V_CW + l * 66:V_CW + (l + 1) * 66]
            cb = vecs2[:, l * NFC:(l + 1) * NFC]
            gt = modT[:, l, 40:48, b]
            for g in range(NG):
                t0 = g * 512
                norm_to(hT, t0, 512, gsT[:, l, b, 1, :], modT[:, l, 24:32, b], sqb, rt, tmpb)
                for fc in range(NRING):
                    S.dma("pool", ring[fc], f_w_out[l][fc * 128:(fc + 1) * 128, :])
                for fc in range(NFC):
                    ups, vps = P[1 + (fc % 3) * 2], P[2 + (fc % 3) * 2]
                    for kc in range(8):
                        S.mm(ups, w_in[:, kc, fc * 128:(fc + 1) * 128], hT[:, kc, :], start=(kc == 0), stop=(kc == 7))
                    for kc in range(8):
                        S.mm(vps, w_in[:, kc, DFF + fc * 128:DFF + (fc + 1) * 128], hT[:, kc, :], start=(kc == 0), stop=(kc == 7))
                    t1 = usb[fc % 2]
                    tc = tcb[fc % 2]
                    gl = glb[fc % 2]
                    w2, w1, w0 = cw[:, 2 * NFC + fc:2 * NFC + fc + 1], cw[:, NFC + fc:NFC + fc + 1], cw[:, fc:fc + 1]
                    S.act(t1[:, 0:512], ups, AF.Identity, scale=w2, bias=cb[:, fc:fc + 1])
                    S.stt("dve", tc[:, 1:512], ups[:, 0:511], w1, t1[:, 1:512], ALU.mult, ALU.add)
                    S.stt("dve", tc[:, 2:512], ups[:, 0:510], w0, tc[:, 2:512], ALU.mult, ALU.add)
                    if g == 0:
                        S.copy("dve", tc[:, 0:1], t1[:, 0:1])
                    else:
                        S.stt("dve", tc[:, 0:1], carry[:, fc, 1:2], w1, t1[:, 0:1], ALU.mult, ALU.add)
                        S.stt("dve", tc[:, 0:2], carry[:, fc, 0:2], w0, tc[:, 0:2], ALU.mult, ALU.add)
                    if g < NG - 1:
                        S.copy("dve", carry[:, fc, :], ups[:, 510:512])
                    S.act(gl, tc, AF.Gelu)
                    S.tt("dve", gT[:, fc, :], gl, vps, ALU.mult)
                for fc in range(NFC):
                    if fc >= NRING:
                        S.dma("pool", ring[fc % NRING], f_w_out[l][fc * 128:(fc + 1) * 128, :])
                    for ncx in range(8):
                        S.mm(P[ncx], ring[fc % NRING][:, ncx * 128:(ncx + 1) * 128], gT[:, fc, :], start=(fc == 0), stop=(fc == NFC - 1))
                for ncx in range(8):
                    S.stt("dve", xT[:, ncx, t0:t0 + 512], P[ncx], gt[:, ncx:ncx + 1], xT[:, ncx, t0:t0 + 512], ALU.mult, ALU.add)
            S.barrier()

    def dsa_phase(b, l):
        with ExitStack() as es:
            A = lambda name, shape, dt=F32: es.enter_context(nc.sbuf_tensor(name + uid(), shape, dt)).ap()
            w_in = A("a_win", [128, 8, 584], BF16)
            w_k2 = A("a_wk2", [128, 8, 128], BF16)
            w_uq = A("a_wuq", [128, 2, 1024], BF16)
            w_uk = A("a_wuk", [128, 8, 256], BF16)
            w_uv = A("a_wuv", [128, 8, 2, 128], BF16)
            w_qi = A("a_wqi", [128, 2, 512], BF16)
            w_o = A("a_wo", [128, 8, 1024], BF16)
            ckvT = A("a_ckvT", [128, 2, T], BF16)
            ckv_tok = A("a_ckvtok", [128, NBLK, 256], BF16)
            kidxT = A("a_kidxT", [128, T], BF16)
            hT = A("a_hT", [128, 8, 512], BF16)
            sqb = [A(f"a_sq{i}", [128, 512], BF16) for i in range(2)]
            tmpb = [A(f"a_tmp{i}", [128, 512], F32) for i in range(2)]
            rt = A("a_rt", [128, 512])
            raw = A("a_raw", [128, 2, 512])
            sq2 = A("a_sq2", [128, 2, 512], BF16)
            rt2 = A("a_rt2", [128, 512])
            cqT = A("a_cqT", [128, 2, 512], BF16)
            widx = A("a_widx", [128, 4, 8])
            qTb = A("a_qTb", [128, 8, 128], BF16)
            qlatT = [A(f"a_qlatT{i}", [128, 2, 1024], BF16) for i in range(2)]
            qidxT = A("a_qidxT", [128, 4, 128], BF16)
            score = hT.bitcast(F32).rearrange("p a b -> p (a b)")[:, 0:T]
            Rb = sqb
            Dg = sq2.rearrange("p a (b c) -> p (a b) c", c=128)
            sel = A("a_sel", [128, T], BF16)
            selT = [A(f"a_selT{i}", [128, NBLK, 128], BF16) for i in range(2)]
            bis = A("a_bis", [128, 8])
            Eb = [A(f"a_E{i}", [128, 512], BF16) for i in range(2)]
            Ssb = A("a_Ssb", [128, 512])
            PTb = [A(f"a_PT{i}", [128, 512], BF16) for i in range(2)]
            rec = Ssb
            olatT = A("a_olatT", [128, 2, 1024], BF16)
            oTb = A("a_oTb", [128, 8, 128], BF16)
            BT = A("a_BT", [128, 2, 1024])
            for dl in range(2):
                src = bass.AP(bvp.tensor, 128 * dl + 127, [[8 * 384 - 1, 128], [384, 8], [1, 128]])
                S.dma("sp", BT[:, dl, :].rearrange("p (h q) -> p h q", h=8), src)

            S.dma("pool", w_in, a_w_in.rearrange("(k p) n -> p k n", p=128))
            for hf in range(2):
                S.dma("pool", w_k2[:, :, hf * 64:(hf + 1) * 64], a_w_in[:, 512:576].rearrange("(k p) n -> p k n", p=128))
            S.dma("pool", w_uq, a_w_uq.rearrange("(k p) n -> p k n", p=128))
            S.dma("pool", w_uk, a_w_uk.rearrange("h d c -> d h c"))
            S.dma("pool", w_uv, a_w_uv.rearrange("h (cc c) v -> c h cc v", cc=2))
            S.dma("pool", w_qi, a_w_qi.rearrange("(k p) n -> p k n", p=128))
            S.dma("pool", w_o, a_w_o.rearrange("(k p) n -> p k n", p=128))
            gcq = vecs[:, V_GCQ:V_GCQ + 2]
            gckv = vecs[:, V_GCKV:V_GCKV + 2]
            gt = modT[:, l, 16:24, b]
            st = {"nE": 0}

            def lat_norm(col0, gvec, dst_fn):
                for oc in range(2):
                    ps = P[5 + oc]
                    for kc in range(8):
                        S.mm(ps, w_in[:, kc, col0 + oc * 128:col0 + (oc + 1) * 128], hT[:, kc, :], start=(kc == 0), stop=(kc == 7))
                    S.copy("dve", raw[:, oc, :], ps)
                    S.act(sq2[:, oc, :], ps, AF.Square)
                for oc in range(2):
                    S.mm(P[7], ones256_bf, sq2[:, oc, :], start=(oc == 0), stop=(oc == 1))
                S.act(rt2, P[7], AF.Sqrt, bias=eps_t)
                S.op("dve", "reciprocal", rt2, rt2, r=[rt2], w=[rt2])
                for oc in range(2):
                    S.stt("dve", dst_fn(oc), raw[:, oc, :], gvec[:, oc:oc + 1], rt2, ALU.mult, ALU.mult)

            def group_level(g):
                t0 = g * 512
                norm_to(hT, t0, 512, gsT[:, l, b, 0, :], modT[:, l, 0:8, b], sqb, rt, tmpb, bank=7)
                lat_norm(0, gcq, lambda oc: cqT[:, oc, :])
                lat_norm(256, gckv, lambda oc: ckvT[:, oc, t0:t0 + 512])
                for kc in range(8):
                    S.mm(P[5], w_k2[:, kc, :], hT[:, kc, :], start=(kc == 0), stop=(kc == 7))
                S.copy("act", kidxT[:, t0:t0 + 512], P[5])
                for j in range(4):
                    for kc in range(8):
                        S.mm(P[6][:, j * 8:(j + 1) * 8], hT[:, kc, j * 128:(j + 1) * 128], w_in[:, kc, 576:584], start=(kc == 0), stop=(kc == 7))
                S.copy("dve", widx.rearrange("p a b -> p (a b)"), P[6][:, 0:32])
                for j in range(4):
                    for cc in range(2):
                        S.mm(P[5 + j // 2][:, ((j % 2) * 2 + cc) * 128:((j % 2) * 2 + cc + 1) * 128], ckvT[:, cc, t0 + j * 128:t0 + (j + 1) * 128], ident_bf)
                for j2 in range(2):
                    S.copy("act", ckv_tok[:, 4 * g + 2 * j2:4 * g + 2 * j2 + 2, :].rearrange("p a c -> p (a c)"), P[5 + j2])

            def sel_part(g, j):
                n = 4 * g + j
                Tk = (n + 1) * 128
                tq = n * 128
                tql = j * 128
                ql = qlatT[n % 2]
                sT = selT[n % 2]
                for h in range(8):
                    ps = P[5 + h // 4]
                    for cc in range(2):
                        S.mm(ps[:, (h % 4) * 128:(h % 4 + 1) * 128], w_uq[:, cc, h * 128:(h + 1) * 128], cqT[:, cc, tql:tql + 128],
                             start=(cc == 0), stop=(cc == 1))
                for q2 in range(2):
                    S.copy("act", qTb[:, q2 * 4:(q2 + 1) * 4, :].rearrange("p a c -> p (a c)"), P[5 + q2])
                for cc in range(2):
                    for h in range(8):
                        ps = P[5 + h // 4]
                        S.mm(ps[:, (h % 4) * 128:(h % 4 + 1) * 128], w_uk[:, h, cc * 128:(cc + 1) * 128], qTb[:, h, :])
                    for q2 in range(2):
                        S.act(ql[:, cc, q2 * 512:(q2 + 1) * 512], P[5 + q2], AF.Identity, scale=128 ** -0.5)
                for pr in range(4):
                    for cc in range(2):
                        S.mm(P[7][:, pr * 128:(pr + 1) * 128], w_qi[:, cc, pr * 128:(pr + 1) * 128], cqT[:, cc, tql:tql + 128],
                             start=(cc == 0), stop=(cc == 1))
                S.copy("act", qidxT.rearrange("p a c -> p (a c)"), P[7])
                for h in range(8):
                    S.ts("pool", Dg[:, h, :], ident_bf, widx[:, j, h:h + 1], None, ALU.mult)
                items = [(kk, h) for kk in range((Tk + 511) // 512) for h in range(8)]

                def idx_mm(i):
                    kk, h = items[i]
                    w = min(512, Tk - kk * 512)
                    pr, hf = h // 2, h % 2
                    S.mm(P[5 + i % 2][:, 0:w], qidxT[64 * hf:64 * hf + 64, pr, :], kidxT[64 * hf:64 * hf + 64, kk * 512:kk * 512 + w])

                idx_mm(0)
                for i in range(len(items)):
                    kk, h = items[i]
                    w = min(512, Tk - kk * 512)
                    if i + 1 < len(items):
                        idx_mm(i + 1)
                    R = Rb[i % 2]
                    S.act(R[:, 0:w], P[5 + i % 2][:, 0:w], AF.Relu)
                    S.mm(P[7][:, 0:w], Dg[:, h, :], R[:, 0:w], start=(h == 0), stop=(h == 7))
                    if h == 7:
                        S.copy("act", score[:, kk * 512:kk * 512 + w], P[7][:, 0:w])
                sv = score[:, 0:Tk]
                if n >= NB0:
                    S.op("dve", "tensor_reduce", bis[:, 0:1], sv, AX.X, ALU.max, r=[sv], w=[bis[:, 0:1]])
                    S.op("dve", "tensor_reduce", bis[:, 1:2], sv, AX.X, ALU.min, r=[sv], w=[bis[:, 1:2]])
                    S.tt("dve", bis[:, 2:3], bis[:, 0:1], bis[:, 1:2], ALU.subtract)
                S.tt("dve", score[:, tq:tq + 128], score[:, tq:tq + 128], cmask, ALU.add)

            def bis_gen(g, j):
                n = 4 * g + j
                Tk = (n + 1) * 128
                sv = score[:, 0:Tk]
                if n >= NB0:
                    S.ts("dve", bis[:, 3:4], bis[:, 2:3], 0.5, bis[:, 1:2], ALU.mult, op1=ALU.add)
                    for it in range(1, NIT + 1):
                        f = 2.0 ** -it
                        S.ts("dve", sel[:, 0:Tk], sv, bis[:, 3:4], None, ALU.is_ge, op1=ALU.add, accum_out=bis[:, 4:5])
                        if it < NIT:
                            S.ts("dve", bis[:, 5:6], bis[:, 4:5], float(NSEL), f, ALU.is_ge, op1=ALU.mult)
                            S.stt("dve", bis[:, 3:4], bis[:, 5:6], bis[:, 2:3], bis[:, 3:4], ALU.mult, ALU.add)
                            S.stt("dve", bis[:, 3:4], bis[:, 2:3], -f / 2, bis[:, 3:4], ALU.mult, ALU.add)
                        else:
                            S.ts("dve", bis[:, 5:6], bis[:, 4:5], float(NSEL), -1.0, ALU.is_ge, op1=ALU.add)
                            S.ts("dve", bis[:, 5:6], bis[:, 5:6], f, None, ALU.mult)
                            S.stt("dve", bis[:, 1:2], bis[:, 5:6], bis[:, 2:3], bis[:, 3:4], ALU.mult, ALU.add)
                        yield
                    S.ts("dve", sel[:, 0:Tk], sv, bis[:, 1:2], None, ALU.is_ge)
                else:
                    S.ts("dve", sel[:, 0:Tk], sv, -1e29, None, ALU.is_ge)
                yield

            def att_part(g, j, gen):
                n = 4 * g + j
                tq = n * 128
                ql = qlatT[n % 2]
                sT = selT[n % 2]
                for k0 in range(0, n + 1, 4):
                    cntc = min(4, n + 1 - k0)
                    pb = P[5 + (k0 // 4) % 2]
                    for kc in range(k0, k0 + cntc):
                        S.mm(pb[:, (kc - k0) * 128:(kc - k0 + 1) * 128], sel[:, kc * 128:(kc + 1) * 128], ident_bf)
                    S.copy("act", sT[:, k0:k0 + cntc, :].rearrange("p a c -> p (a c)"), pb[:, 0:cntc * 128])
                steps = [(hh, kc) for hh in range(2) for kc in range(n + 1)]
                base = st["nE"]
                st["nE"] += len(steps)

                def qk(i):
                    hh, kc = steps[i]
                    Sps = P[1 + (base + i) % 2]
                    for cc in range(2):
                        S.mm(Sps, ckvT[:, cc, kc * 128:(kc + 1) * 128], ql[:, cc, hh * 512:(hh + 1) * 512], start=(cc == 0), stop=(cc == 1))

                qk(0)
                for i in range(len(steps)):
                    hh, kc = steps[i]
                    if i + 1 < len(steps):
                        qk(i + 1)
                    Sps = P[1 + (base + i) % 2]
                    E = Eb[(base + i) % 2]
                    PTt = PTb[(base + i) % 2]
                    dl = n - kc
                    if dl <= 1:
                        S.tt("dve", Ssb, Sps, BT[:, dl, hh * 512:(hh + 1) * 512], ALU.add)
                        S.act(E, Ssb, AF.Exp)
                    else:
                        S.act(E, Sps, AF.Exp)
                    S.tt("pool", PTt.rearrange("p (h q) -> p h q", h=4), E.rearrange("p (h q) -> p h q", h=4), fbc(sT[:, kc, :], 4), ALU.mult)
                    for cc in range(2):
                        S.mm(P[3 + cc], ckv_tok[:, kc, cc * 128:(cc + 1) * 128], PTt, start=(kc == 0), stop=(kc == n))
                    S.mm(P[0], ones_bf, PTt, start=(kc == 0), stop=(kc == n))
                    if kc == n:
                        S.act(rec, P[0], AF.Ln)
                        S.act(rec, rec, AF.Exp, scale=-1.0)
                        for cc in range(2):
                            S.tt("dve", olatT[:, cc, hh * 512:(hh + 1) * 512], P[3 + cc], rec, ALU.mult)
                    if gen is not None:
                        next(gen, None)
                if gen is not None:
                    for _ in gen:
                        pass
                for h in range(8):
                    ps = P[1 + h // 4]
                    for cc in range(2):
                        S.mm(ps[:, (h % 4) * 128:(h % 4 + 1) * 128], w_uv[:, h, cc, :], olatT[:, cc, h * 128:(h + 1) * 128], start=(cc == 0), stop=(cc == 1))
                for q2 in range(2):
                    S.copy("act", oTb[:, q2 * 4:(q2 + 1) * 4, :].rearrange("p a c -> p (a c)"), P[1 + q2])
                for ncx in range(8):
                    ps = P[3 + ncx // 4]
                    for h in range(8):
                        S.mm(ps[:, (ncx % 4) * 128:(ncx % 4 + 1) * 128], w_o[:, h, ncx * 128:(ncx + 1) * 128], oTb[:, h, :], start=(h == 0), stop=(h == 7))
                for ncx in range(8):
                    ps = P[3 + ncx // 4]
                    S.stt("dve", xT[:, ncx, tq:tq + 128], ps[:, (ncx % 4) * 128:(ncx % 4 + 1) * 128], gt[:, ncx:ncx + 1], xT[:, ncx, tq:tq + 128], ALU.mult, ALU.add)

            prev = None
            for g in range(NG):
                group_level(g)
                for j in range(4):
                    sel_part(g, j)
                    gen = bis_gen(g, j)
                    if prev is None:
                        for _ in gen:
                            pass
                    else:
                        att_part(prev[0], prev[1], gen)
                    prev = (g, j)
            att_part(prev[0], prev[1], None)
            S.barrier()

    def gla_phase(b, l):
        with ExitStack() as es:
            A = lambda name, shape, dt=F32: es.enter_context(nc.sbuf_tensor(name + uid(), shape, dt)).ap()
            w_in = A("b_win", [128, 8, 3088], BF16)
            w_o = A("b_wo", [128, 8, 1024], BF16)
            w_g2 = A("b_wg2", [32, 512])
            hT = A("b_hT", [128, 8, 512], BF16)
            sqb = [A(f"b_sq{i}", [128, 512], BF16) for i in range(2)]
            tmpb = [A(f"b_tmp{i}", [128, 512], F32) for i in range(2)]
            rt = A("b_rt", [128, 512])
            qraw = A("b_qraw", [128, 4, 512])
            kraw = A("b_kraw", [128, 4, 512])
            rs = A("b_rs", [128, 8, 512], BF16)
            rtmp = tmpb
            glr = A("b_glr", [32, 512])
            ogT = A("b_ogT", [128, 8, 512], BF16)
            L = A("b_L", [128, 512])
            E1 = A("b_E1", [128, 512])
            E2 = A("b_E2", [128, 512])
            E3 = L
            qt = A("b_qt", [128, 4, 128], BF16)
            kt = A("b_kt", [128, 4, 128], BF16)
            kh = A("b_kh", [128, 512], BF16)
            vb = A("b_vb", [128, 1024], BF16)
            AT = A("b_AT", [128, 4, 128], BF16)
            St = A("b_S", [128, 4, 256])
            Sbf = A("b_Sbf", [128, 4, 256], BF16)
            on = E2.bitcast(BF16)
            ssq = A("b_ssq", [128, 8])
            junk = rt[:, 0:256]

            S.dma("pool", w_in, b_w_in.rearrange("(k p) n -> p k n", p=128))
            S.dma("pool", w_o, b_w_o.rearrange("(k p) n -> p k n", p=128))
            S.dma("sp", w_g2[0:16, :], b_w_g2)
            S.dma("sp", w_g2[16:17, :], b_b_g)
            S.memset("dve", St, 0.0)
            S.memset("dve", Sbf, 0.0)
            S.memset("dve", glr, 1.0)
            gn = vecs[:, V_GNORM:V_GNORM + 8]
            gt = modT[:, l, 16:24, b]
            for g in range(NG):
                t0 = g * 512
                norm_to(hT, t0, 512, gsT[:, l, b, 0, :], modT[:, l, 0:8, b], sqb, rt, tmpb)
                for h in range(4):
                    for (dst, c0) in ((qraw, 0), (kraw, 512)):
                        ps = P[1 + (h % 2)] if dst is qraw else P[3 + (h % 2)]
                        for kc in range(8):
                            S.mm(ps, w_in[:, kc, c0 + h * 128:c0 + (h + 1) * 128], hT[:, kc, :], start=(kc == 0), stop=(kc == 7))
                        evac(dst[:, h, :], ps)
                for c8 in range(8):
                    ps = P[5 + c8 % 2]
                    for kc in range(8):
                        S.mm(ps, w_in[:, kc, 2048 + c8 * 128:2048 + (c8 + 1) * 128], hT[:, kc, :], start=(kc == 0), stop=(kc == 7))
                    S.act(rtmp[c8 % 2], ps, AF.Silu)
                    S.ts("dve", rs[:, c8, :], rtmp[c8 % 2], gn[:, c8:c8 + 1], None, ALU.mult)
                for kc in range(8):
                    S.mm(P[0][0:16, :], w_in[:, kc, 3072:3088], hT[:, kc, :], start=(kc == 0), stop=(kc == 7))
                S.copy("dve", glr[0:16, :], P[0][0:16, :])
                for j in range(4):
                    tl = j * 128
                    tq = t0 + tl
                    for kc in range(8):
                        S.mm(P[1], hT[:, kc, tl:tl + 128], w_in[:, kc, 512:1024], start=(kc == 0), stop=(kc == 7))
                    for vh in range(2):
                        for kc in range(8):
                            S.mm(P[2 + vh], hT[:, kc, tl:tl + 128], w_in[:, kc, 1024 + vh * 512:1024 + (vh + 1) * 512], start=(kc == 0), stop=(kc == 7))
                    S.mm(P[4], glr[0:17, tl:tl + 128], w_g2[0:17, :])
                    S.act(L, P[4], AF.Exp, scale=-1.0)
                    S.act(L, L, AF.Ln, bias=1.0)
                    for h in range(4):
                        S.mm(P[5][:, h * 128:(h + 1) * 128], L[:, h * 128:(h + 1) * 128], TRI)
                    S.mm(P[6], TRIR, L)
                    S.act(E1, P[5], AF.Exp)
                    S.act(E2, P[5], AF.Exp, scale=-1.0)
                    S.act(E3, P[6], AF.Exp)
                    S.stt("dve", qt, qraw[:, :, tl:tl + 128], 128 ** -0.5, E1.rearrange("p (h t) -> p h t", h=4), ALU.mult, ALU.mult)
                    S.tt("dve", kt, kraw[:, :, tl:tl + 128], E2.rearrange("p (h t) -> p h t", h=4), ALU.mult)
                    S.tt("dve", kh, P[1], E3, ALU.mult)
                    S.copy("act", vb[:, 0:512], P[2])
                    S.copy("act", vb[:, 512:1024], P[3])
                    for h in range(4):
                        S.mm(P[4][:, h * 128:(h + 1) * 128], kt[:, h, :], qt[:, h, :])
                    S.tt("dve", AT, P[4].rearrange("p (h t) -> p h t", h=4), fbc(triT_bf, 4), ALU.mult)
                    for h in range(4):
                        po = P[1 + h // 2][:, (h % 2) * 256:(h % 2 + 1) * 256]
                        S.mm(po, qt[:, h, :], Sbf[:, h, :], start=True, stop=False)
                        S.mm(po, AT[:, h, :], vb[:, h * 256:(h + 1) * 256], start=False, stop=True)
                    for h in range(4):
                        pn = P[5 + h // 2][:, (h % 2) * 256:(h % 2 + 1) * 256]
                        S.mm(pn, kh[:, h * 128:(h + 1) * 128], vb[:, h * 256:(h + 1) * 256])
                    for h in range(4):
                        pn = P[5 + h // 2][:, (h % 2) * 256:(h % 2 + 1) * 256]
                        S.stt("dve", St[:, h, :], St[:, h, :], E1[:, h * 128 + 127:h * 128 + 128], pn, ALU.mult, ALU.add)
                        S.copy("act", Sbf[:, h, :], St[:, h, :])
                    S.memset("dve", ssq[:, 0:4], 0.0)
                    for h in range(4):
                        po = P[1 + h // 2][:, (h % 2) * 256:(h % 2 + 1) * 256]
                        S.act(junk, po, AF.Square, accum_out=ssq[:, h:h + 1])
                    S.act(ssq[:, 4:8], ssq[:, 0:4], AF.Sqrt, bias=eps_t, scale=1.0 / 256)
                    S.op("dve", "reciprocal", ssq[:, 4:8], ssq[:, 4:8], r=[ssq[:, 4:8]], w=[ssq[:, 4:8]])
                    for h in range(4):
                        po = P[1 + h // 2][:, (h % 2) * 256:(h % 2 + 1) * 256]
                        S.ts("dve", on[:, h * 256:(h + 1) * 256], po, ssq[:, 4 + h:5 + h], None, ALU.mult)
                    for c8 in range(8):
                        S.mm(P[0 if c8 < 4 else 7][:, (c8 % 4) * 128:(c8 % 4 + 1) * 128], on[:, c8 * 128:(c8 + 1) * 128], ident_bf)
                    for q2 in range(2):
                        S.tt("dve", ogT[:, q2 * 4:(q2 + 1) * 4, tl:tl + 128], P[0 if q2 == 0 else 7].rearrange("p (c t) -> p c t", c=4), rs[:, q2 * 4:(q2 + 1) * 4, tl:tl + 128], ALU.mult)
                for half in range(2):
                    for n4 in range(4):
                        ncx = half * 4 + n4
                        for c8 in range(8):
                            S.mm(P[1 + n4], w_o[:, c8, ncx * 128:(ncx + 1) * 128], ogT[:, c8, :], start=(c8 == 0), stop=(c8 == 7))
                    for n4 in range(4):
                        ncx = half * 4 + n4
                        S.stt("dve", xT[:, ncx, t0:t0 + 512], P[1 + n4], gt[:, ncx:ncx + 1], xT[:, ncx, t0:t0 + 512], ALU.mult, ALU.add)
            S.barrier()

    def final_out(b, out=out):
        with ExitStack() as es:
            A = lambda name, shape, dt=F32: es.enter_context(nc.sbuf_tensor(name + uid(), shape, dt)).ap()
            hN = A("o_hN", [128, 8, 512])
            sqb = [A(f"o_sq{i}", [128, 512], BF16) for i in range(2)]
            tmpb = [A(f"o_tmp{i}", [128, 512], F32) for i in range(2)]
            rt = A("o_rt", [128, 512])
            ob = [A(f"o_ob{i}", [128, D]) for i in range(4)]
            gf = vecs[:, V_GFIN:V_GFIN + 8]
            for g in range(NG):
                t0 = g * 512
                norm_to(hN, t0, 512, gf, None, sqb, rt, tmpb)
                for j in range(4):
                    o_ = ob[j % 4]
                    for q4 in range(2):
                        pb = P[1 + (j * 2 + q4) % 4]
                        for kk in range(4):
                            kc = q4 * 4 + kk
                            S.transpose(pb[:, kk * 128:(kk + 1) * 128], hN[:, kc, j * 128:(j + 1) * 128], ident)
                        evac(o_[:, q4 * 512:(q4 + 1) * 512], pb)
                    S.dma("sp", out[b, t0 + j * 128:t0 + (j + 1) * 128, :], o_)
            S.barrier()

    for b in range(NB):
        load_x(b)
        if dbg:
            final_out(b, douts[0])
        if nst >= 2:
            dsa_phase(b, 0)
            if dbg:
                final_out(b, douts[1])
        if nst >= 3:
            ffn_phase(b, 0)
            if dbg:
                final_out(b, douts[2])
        if nst >= 4:
            gla_phase(b, 1)
            if dbg:
                final_out(b, douts[3])
        if nst >= 5:
            ffn_phase(b, 1)
        final_out(b)
    n = S.emit()
    return nc, n


def prep_inputs(inp, b0, NB, T):
    f = lambda a: np.ascontiguousarray(np.asarray(a, dtype=np.float32))
    m = {
        "x": f(inp["x"][b0:b0 + NB, :T]), "c": f(inp["c"][b0:b0 + NB]), "rel_bias": f(inp["rel_bias"]),
        "a_w_in": f(inp["a_w_in"][0]), "a_g_cq": f(inp["a_g_cq"][0].reshape(2, 128)), "a_g_ckv": f(inp["a_g_ckv"][0].reshape(2, 128)),
        "a_w_uq": f(inp["a_w_uq"][0]), "a_w_uk": f(inp["a_w_uk"][0]), "a_w_uv": f(inp["a_w_uv"][0]),
        "a_w_qi": f(inp["a_w_qi"][0]), "a_w_o": f(inp["a_w_o"][0]),
        "b_w_in": f(inp["b_w_in"][0]), "b_w_g2": f(inp["b_w_g2"][0]), "b_b_g": f(inp["b_b_g"][0].reshape(1, 512)),
        "b_g_norm": f(inp["b_g_norm"][0].reshape(8, 128)), "b_w_o": f(inp["b_w_o"][0]),
        "ada_w": f(inp["ada_w"]), "ada_b": f(inp["ada_b"].reshape(96, 128)),
        "g_mix": f(inp["g_mix"].reshape(16, 128)), "g_ffn": f(inp["g_ffn"].reshape(16, 128)),
        "f_w_in": f(inp["f_w_in"]), "f_conv_w": f(inp["f_conv_w"].reshape(2, 66, 128)), "f_conv_b": f(inp["f_conv_b"].reshape(44, 128)),
        "f_w_out": f(inp["f_w_out"]), "g_final": f(inp["g_final"].reshape(8, 128)),
        "consts": make_consts(),
    }
    return m


_T = 2048
_NB = 2
_NCORES = 8


def kernel(**inputs):
    inp = {k: np.asarray(v) for k, v in inputs.items()}
    nc, _ = build(T=_T, NB=_NB)
    in_maps = [prep_inputs(inp, core * _NB, _NB, _T) for core in range(_NCORES)]
    res = run_bass_kernel_spmd(nc, in_maps, core_ids=list(range(_NCORES)))
    outs = [np.asarray(r["out"], dtype=np.float32) for r in res.results]
    return np.concatenate(outs, axis=0)
```
